# Optimizing a Trainium2 kernel written in Bass

```python
import jax
import jax.numpy as jnp
from jax import lax
import numpy as np

D_MODEL = 1024
BATCH = 8
SEQ = 4096
DEPTH = 2

A_HEADS = 8
A_HEAD_DIM = 64
A_OUT = A_HEADS * A_HEAD_DIM
IDX_HEADS = 4
IDX_DIM = 64
TOPK_MAX = 256
Q_BLOCK = 128
ROPE_THETA = 500000.0
ROT_DIM = A_HEAD_DIM // 4
B_HEADS = 4
B_KEY_DIM = 64
B_VAL_DIM = 128
B_OUT = B_HEADS * B_VAL_DIM
B_GATE_RANK = 16
B_GATE_TAU = 16.0
B_CHUNK = 64
C_GROUPS = 4
C_GROUP_DIM = 128
C_OUT = C_GROUPS * C_GROUP_DIM
C_CHUNK = 128
N_GROUPS = 4
EXPERTS_PER_GROUP = 8
N_EXPERTS = N_GROUPS * EXPERTS_PER_GROUP
D_EXPERT = 256
TOP_K_INNER = 2
DN_ALPHA = (2 * DEPTH) ** 0.25
DN_BETA = (8 * DEPTH) ** -0.25
LN_EPS = 1e-5
RMS_EPS = 1e-6

SPLIT_WIDTHS = (A_OUT, A_HEAD_DIM, A_HEAD_DIM,
                IDX_HEADS * IDX_DIM, IDX_DIM, IDX_HEADS,
                B_HEADS * B_KEY_DIM, B_HEADS * B_KEY_DIM,
                B_OUT, B_GATE_RANK, B_OUT,
                2 * C_OUT,
                D_MODEL, D_MODEL, D_MODEL)
N_IN = sum(SPLIT_WIDTHS)

kernel_name = 'hybrid_dsa_gla_gmlp_hmoe'


def layer_norm(x, g, b=None):
    xf = x.astype(jnp.float32)
    mu = jnp.mean(xf, axis=-1, keepdims=True)
    var = jnp.mean(jnp.square(xf - mu), axis=-1, keepdims=True)
    y = (xf - mu) * lax.rsqrt(var + LN_EPS) * g.astype(jnp.float32)
    if b is not None:
        y = y + b.astype(jnp.float32)
    return y.astype(x.dtype)


def rms_norm(x, g):
    xf = x.astype(jnp.float32)
    y = xf * lax.rsqrt(jnp.mean(jnp.square(xf), axis=-1, keepdims=True) + RMS_EPS)
    return (y * g.astype(jnp.float32)).astype(x.dtype)


def rope_tables(positions):
    inv_freq = ROPE_THETA ** (-jnp.arange(0, ROT_DIM, 2, dtype=jnp.float32) / ROT_DIM)
    ang = positions.astype(jnp.float32)[..., None] * inv_freq
    return jnp.cos(ang), jnp.sin(ang)


def apply_partial_rope(x, cos, sin):
    half = ROT_DIM // 2
    c = cos[:, :, None, :].astype(x.dtype)
    s = sin[:, :, None, :].astype(x.dtype)
    x1 = x[..., :half]
    x2 = x[..., half:ROT_DIM]
    return jnp.concatenate([x1 * c - x2 * s, x1 * s + x2 * c, x[..., ROT_DIM:]], axis=-1)


def dsa_attention(q, k, v, qi, ki, wi):
    f32 = jnp.float32
    bsz, seq_len = q.shape[0], q.shape[1]
    n_sel = min(TOPK_MAX, seq_len // 4)
    n_blk = seq_len // Q_BLOCK
    scale = A_HEAD_DIM ** -0.5
    key_pos = jnp.arange(seq_len, dtype=jnp.int32)
    ki32 = ki.astype(f32)

    def to_blocks(t):
        return jnp.moveaxis(t.reshape((bsz, n_blk, Q_BLOCK) + t.shape[2:]), 1, 0)

    def block_fn(args):
        qb, qib, wib, start = args
        qpos = start + jnp.arange(Q_BLOCK, dtype=jnp.int32)
        causal = key_pos[None, :] <= qpos[:, None]
        dots = jnp.einsum('bqhd,bsd->bqhs', qib.astype(f32), ki32)
        score = jnp.einsum('bqhs,bqh->bqs', jax.nn.relu(dots), wib.astype(f32))
        score = jnp.where(causal[None], score, -jnp.inf)
        _, sel = lax.top_k(score, n_sel)
        valid = sel <= qpos[None, :, None]
        k_sel = jax.vmap(lambda kk, ii: kk[ii])(k, sel)
        v_sel = jax.vmap(lambda vv, ii: vv[ii])(v, sel)
        logits = jnp.einsum('bqhd,bqkd->bqhk', qb, k_sel).astype(f32) * scale
        logits = jnp.where(valid[:, :, None, :], logits, -jnp.inf)
        p = jax.nn.softmax(logits, axis=-1)
        return jnp.einsum('bqhk,bqkd->bqhd', p.astype(v.dtype), v_sel)

    starts = jnp.arange(n_blk, dtype=jnp.int32) * Q_BLOCK
    out = lax.map(block_fn, (to_blocks(q), to_blocks(qi), to_blocks(wi), starts))
    return jnp.moveaxis(out, 0, 1).reshape(bsz, seq_len, A_OUT)


def gla_chunked(q, k, v, log_a):
    f32 = jnp.float32
    bsz, seq_len, n_h, dk = q.shape
    dv = v.shape[-1]
    n_c = seq_len // B_CHUNK

    def chunks(t):
        return t.astype(f32).reshape(bsz, n_c, B_CHUNK, n_h, t.shape[-1]).transpose(1, 0, 3, 2, 4)

    tri = jnp.tril(jnp.ones((B_CHUNK, B_CHUNK), dtype=bool))

    def step(state, inp):
        qc, kc, vc, gc = inp
        b = jnp.cumsum(gc, axis=2)
        diff = b[:, :, :, None, :] - b[:, :, None, :, :]
        decay = jnp.exp(jnp.where(tri[:, :, None], diff, -jnp.inf))
        att = jnp.einsum('bhtk,bhtsk,bhsk->bhts', qc, decay, kc)
        o = jnp.einsum('bhts,bhsv->bhtv', att, vc) + jnp.einsum('bhtk,bhkv->bhtv', qc * jnp.exp(b), state)
        b_last = b[:, :, -1:, :]
        state = jnp.exp(b_last[:, :, 0, :])[..., None] * state + jnp.einsum('bhsk,bhsv->bhkv', kc * jnp.exp(b_last - b), vc)
        return state, o

    s0 = jnp.zeros((bsz, n_h, dk, dv), f32)
    _, o = lax.scan(step, s0, (chunks(q), chunks(k), chunks(v), chunks(log_a)))
    return o.transpose(1, 0, 3, 2, 4).reshape(bsz, seq_len, n_h, dv)


def chunked_spatial_gating(c_uv, ln_g, ln_b, w_s, b_s):
    bsz, seq_len = c_uv.shape[0], c_uv.shape[1]
    z = jax.nn.gelu(c_uv)
    u, v = z[..., :C_OUT], z[..., C_OUT:]
    v = layer_norm(v, ln_g, ln_b)
    n_c = seq_len // C_CHUNK
    vb = v.reshape(bsz, n_c, C_CHUNK, C_GROUPS, C_GROUP_DIM)
    w = w_s * jnp.tril(jnp.ones((C_CHUNK, C_CHUNK), dtype=w_s.dtype))[None]
    mixed = jnp.einsum('gts,bcsgd->bctgd', w, vb) + b_s.T[:, :, None]
    return u * mixed.reshape(bsz, seq_len, C_OUT)


def mixer_sublayer(x, cos, sin, w_in, idx_k_g, gla_wa2, gla_ba, gla_norm_g,
                   gm_ln_g, gm_ln_b, gm_ws, gm_bs, w_branch_a, w_branch_b, w_branch_c, w_out):
    bsz, seq_len, _ = x.shape
    points = np.cumsum(SPLIT_WIDTHS)[:-1].tolist()
    (a_q, a_k, a_v, i_q, i_k, i_w, b_q, b_k, b_v, b_glr, b_r, c_uv,
     g_a, g_b, g_c) = jnp.split(x @ w_in, points, axis=-1)

    q = apply_partial_rope(a_q.reshape(bsz, seq_len, A_HEADS, A_HEAD_DIM), cos, sin)
    k = apply_partial_rope(a_k[:, :, None, :], cos, sin)[:, :, 0]
    qi = apply_partial_rope(i_q.reshape(bsz, seq_len, IDX_HEADS, IDX_DIM), cos, sin)
    ki = apply_partial_rope(layer_norm(i_k, idx_k_g)[:, :, None, :], cos, sin)[:, :, 0]
    wi = i_w * (IDX_HEADS ** -0.5 * IDX_DIM ** -0.5)
    y_a = dsa_attention(q, k, a_v, qi, ki, wi)

    log_a = jax.nn.log_sigmoid((b_glr @ gla_wa2 + gla_ba).astype(jnp.float32)) / B_GATE_TAU
    o_b = gla_chunked(b_q.reshape(bsz, seq_len, B_HEADS, B_KEY_DIM) * (B_KEY_DIM ** -0.5),
                      b_k.reshape(bsz, seq_len, B_HEADS, B_KEY_DIM),
                      b_v.reshape(bsz, seq_len, B_HEADS, B_VAL_DIM),
                      log_a.reshape(bsz, seq_len, B_HEADS, B_KEY_DIM)).astype(x.dtype)
    y_b = jax.nn.silu(b_r) * rms_norm(o_b, gla_norm_g).reshape(bsz, seq_len, B_OUT)

    y_c = chunked_spatial_gating(c_uv, gm_ln_g, gm_ln_b, gm_ws, gm_bs)

    h = (jax.nn.sigmoid(g_a) * (y_a @ w_branch_a)
         + jax.nn.sigmoid(g_b) * (y_b @ w_branch_b)
         + jax.nn.sigmoid(g_c) * (y_c @ w_branch_c))
    return h @ w_out


def moe_sublayer(x, w_rg, b_rg, w_re, b_re, w_gate, w_up, w_down):
    bsz, seq_len, d = x.shape
    xf = x.reshape(-1, d)
    n_tok = xf.shape[0]
    g_prob = jax.nn.softmax((xf @ w_rg + b_rg).astype(jnp.float32), axis=-1)
    g_p, g_idx = lax.top_k(g_prob, 1)
    e_logits = (xf @ w_re + b_re).astype(jnp.float32).reshape(n_tok, N_GROUPS, EXPERTS_PER_GROUP)
    e_in = e_logits[jnp.arange(n_tok), g_idx[:, 0]]
    e_top, e_idx = lax.top_k(e_in, TOP_K_INNER)
    e_w = jax.nn.softmax(e_top, axis=-1) * g_p
    expert_id = g_idx * EXPERTS_PER_GROUP + e_idx
    combine = jnp.sum(jax.nn.one_hot(expert_id, N_EXPERTS, dtype=jnp.float32) * e_w[..., None], axis=1)
    combine = combine.astype(x.dtype).reshape(bsz, seq_len, N_EXPERTS)
    w_down_flat = w_down.reshape(N_EXPERTS * D_EXPERT, d)

    def per_seq(args):
        xs, cs = args
        hid = jax.nn.silu(jnp.einsum('ld,edf->lef', xs, w_gate)) * jnp.einsum('ld,edf->lef', xs, w_up)
        hid = hid * cs[:, :, None]
        return hid.reshape(xs.shape[0], N_EXPERTS * D_EXPERT) @ w_down_flat

    return lax.map(per_seq, (x, combine))


def setup_inputs(seed: int = 0) -> dict:
    key = jax.random.key(seed)
    ks = jax.random.split(key, 32)
    f32 = jnp.float32

    def nrm(k, shape, scale):
        return jax.random.normal(k, shape, f32) * scale

    def gain(k, shape):
        return 1.0 + 0.02 * jax.random.normal(k, shape, f32)

    def small(k, shape, s=0.02):
        return s * jax.random.normal(k, shape, f32)

    L = DEPTH
    x = jax.random.normal(ks[0], (BATCH, SEQ, D_MODEL), f32)
    offs = jax.random.randint(ks[1], (BATCH, 1), 0, 1024, dtype=jnp.int32)
    positions = offs + jnp.arange(SEQ, dtype=jnp.int32)[None, :]
    return {
        'x': x,
        'positions': positions,
        'ln_in_g': gain(ks[2], (D_MODEL,)),
        'ln_in_b': small(ks[3], (D_MODEL,)),
        'w_in': nrm(ks[4], (L, D_MODEL, N_IN), D_MODEL ** -0.5),
        'idx_k_g': gain(ks[5], (L, IDX_DIM)),
        'gla_wa2': nrm(ks[6], (L, B_GATE_RANK, B_HEADS * B_KEY_DIM), B_GATE_RANK ** -0.5),
        'gla_ba': small(ks[7], (L, B_HEADS * B_KEY_DIM), 0.1),
        'gla_norm_g': gain(ks[8], (L, B_VAL_DIM)),
        'gm_ln_g': gain(ks[9], (L, C_OUT)),
        'gm_ln_b': small(ks[10], (L, C_OUT)),
        'gm_ws': nrm(ks[11], (L, C_GROUPS, C_CHUNK, C_CHUNK), 0.5 * C_CHUNK ** -0.5),
        'gm_bs': gain(ks[12], (L, C_GROUPS, C_CHUNK)),
        'w_branch_a': nrm(ks[13], (L, A_OUT, D_MODEL), DN_BETA * A_OUT ** -0.5),
        'w_branch_b': nrm(ks[14], (L, B_OUT, D_MODEL), DN_BETA * B_OUT ** -0.5),
        'w_branch_c': nrm(ks[15], (L, C_OUT, D_MODEL), DN_BETA * C_OUT ** -0.5),
        'w_out': nrm(ks[16], (L, D_MODEL, D_MODEL), DN_BETA * D_MODEL ** -0.5),
        'ln1_g': gain(ks[17], (L, D_MODEL)),
        'ln1_b': small(ks[18], (L, D_MODEL)),
        'w_rg': nrm(ks[19], (L, D_MODEL, N_GROUPS), D_MODEL ** -0.5),
        'b_rg': small(ks[20], (L, N_GROUPS), 0.01),
        'w_re': nrm(ks[21], (L, D_MODEL, N_EXPERTS), D_MODEL ** -0.5),
        'b_re': small(ks[22], (L, N_EXPERTS), 0.01),
        'w_gate': nrm(ks[23], (L, N_EXPERTS, D_MODEL, D_EXPERT), D_MODEL ** -0.5),
        'w_up': nrm(ks[24], (L, N_EXPERTS, D_MODEL, D_EXPERT), D_MODEL ** -0.5),
        'w_down': nrm(ks[25], (L, N_EXPERTS, D_EXPERT, D_MODEL), DN_BETA * D_EXPERT ** -0.5),
        'ln2_g': gain(ks[26], (L, D_MODEL)),
        'ln2_b': small(ks[27], (L, D_MODEL)),
    }


def reference(x, positions, ln_in_g, ln_in_b, w_in, idx_k_g, gla_wa2, gla_ba, gla_norm_g,
              gm_ln_g, gm_ln_b, gm_ws, gm_bs, w_branch_a, w_branch_b, w_branch_c, w_out,
              ln1_g, ln1_b, w_rg, b_rg, w_re, b_re, w_gate, w_up, w_down, ln2_g, ln2_b):
    cos, sin = rope_tables(positions)
    h = layer_norm(x, ln_in_g, ln_in_b)
    for l in range(DEPTH):
        mix = mixer_sublayer(h, cos, sin, w_in[l], idx_k_g[l], gla_wa2[l], gla_ba[l], gla_norm_g[l],
                             gm_ln_g[l], gm_ln_b[l], gm_ws[l], gm_bs[l],
                             w_branch_a[l], w_branch_b[l], w_branch_c[l], w_out[l])
        h = layer_norm(DN_ALPHA * h + mix, ln1_g[l], ln1_b[l])
        ffn = moe_sublayer(h, w_rg[l], b_rg[l], w_re[l], b_re[l], w_gate[l], w_up[l], w_down[l])
        h = layer_norm(DN_ALPHA * h + ffn, ln2_g[l], ln2_b[l])
    return h
```

```python
import numpy as np
from contextlib import ExitStack
import concourse.bass as bass
import concourse.mybir as mybir
from concourse.bass_utils import run_bass_kernel_spmd

F32 = mybir.dt.float32
BF16 = mybir.dt.bfloat16
I32 = mybir.dt.int32
AF = mybir.ActivationFunctionType
ALU = mybir.AluOpType
AX = mybir.AxisListType


class Buf:
    __slots__ = ("name", "last_write", "readers", "dstream")

    def __init__(self, name):
        self.name = name
        self.last_write = None
        self.readers = []
        self.dstream = None


class V:
    __slots__ = ("ap", "buf")

    def __init__(self, ap, buf):
        self.ap = ap
        self.buf = buf

    def __getitem__(self, idx):
        return V(self.ap[idx], self.buf)

    def re(self, pat, **kw):
        return V(self.ap.rearrange(pat, **kw), self.buf)

    def bc(self, shape):
        return V(self.ap.to_broadcast(list(shape)), self.buf)

    def un(self, axis):
        return V(self.ap.unsqueeze(axis), self.buf)

    def wb(self, bufs):
        return V(self.ap, bufs)


class Op:
    __slots__ = ("eng", "fn", "deps", "is_dma", "stream", "signal", "count", "tag")


class Stream:
    def __init__(self, name, inc):
        self.name = name
        self.inc = inc
        self.total = 0
        self.sem = None


NDSEM = 90
STRICT = True
import os
USE_SWDGE = bool(os.environ.get("USE_SWDGE"))
ENGS = ("pe", "act", "dve", "pool", "sp")
ENGOBJ = {"pe": "tensor", "act": "scalar", "dve": "vector", "pool": "gpsimd", "sp": "sync"}


def _bufs(vs):
    out = []
    for v in vs:
        b = v.buf if isinstance(v, V) else v
        if isinstance(b, (tuple, list)):
            for x in b:
                if x not in out:
                    out.append(x)
        elif b not in out:
            out.append(b)
    return out


class Prog:
    def __init__(self, nc):
        self.nc = nc
        self.ges = ExitStack()
        self.estream = {e: Stream("s_" + e, 1) for e in ENGS}
        for e in ENGS:
            self.estream[e].sem = self.ges.enter_context(nc.semaphore("s_" + e))
        self.dsem_pool = [self.ges.enter_context(nc.semaphore("dsem%d" % i)) for i in range(NDSEM)]
        self.dsem_next = 0
        self.dsem_base = [0] * NDSEM
        self.nphase = 0
        self.tot_ops = 0
        self.tot_waits = 0
        self.pes = None

    def _es(self, glob):
        return self.ges if glob else self.pes

    def sbuf(self, name, shape, dtype, nslots=None, glob=False):
        if not glob:
            name = self.pname + "_" + name
        t = self._es(glob).enter_context(self.nc.sbuf_tensor(name, list(shape), dtype))
        if nslots is None:
            return V(t[:], Buf(name))
        return [V(t[:, i], Buf(f"{name}{i}")) for i in range(nslots)]

    def psum(self, name, shape, dtype=F32, nslots=None):
        name = self.pname + "_" + name
        t = self.pes.enter_context(self.nc.psum_tensor(name, list(shape), dtype))
        if nslots is None:
            return V(t[:], Buf(name))
        return [V(t[:, i], Buf(f"{name}{i}")) for i in range(nslots)]

    def dram(self, name, shape, dtype, kind="Internal"):
        t = self.nc.dram_tensor(name, list(shape), dtype, kind=kind)
        return V(t.ap(), Buf(name))

    def begin(self, name):
        self.pname = name
        self.pes = ExitStack()
        self.ops = {e: [] for e in ENGS}
        self.all_ops = []
        self.dstreams = []
        self.touched = []

    def _rec(self, eng, fn, reads, writes, is_dma=False, dbuf=None, tag=""):
        op = Op()
        op.eng = eng
        op.fn = fn
        op.is_dma = is_dma
        op.signal = is_dma
        op.count = 0
        op.tag = tag
        if is_dma:
            b = _bufs([dbuf])[0]
            if b.dstream is None:
                b.dstream = Stream("d%d_%s" % (self.nphase, b.name), 16)
                self.dstreams.append(b.dstream)
                self.touched.append(b)
            op.stream = b.dstream
        else:
            op.stream = self.estream[eng]
        rb = _bufs(reads)
        wb = _bufs(writes)
        deps = []
        for b in rb:
            lw = b.last_write
            if lw is not None:
                deps.append((lw, "raw"))
        for b in wb:
            lw = b.last_write
            if lw is not None:
                deps.append((lw, "waw"))
            for r in b.readers:
                deps.append((r, "war"))
        fdeps = []
        for d, kind in deps:
            if d is op:
                continue
            if (not d.is_dma) and (not is_dma) and d.eng == eng:
                if eng == "pe" or (kind != "raw" and not STRICT):
                    continue
            if d.is_dma and is_dma and d.stream is op.stream and kind == "waw":
                continue
            fdeps.append(d)
        op.deps = fdeps
        for b in rb:
            b.readers.append(op)
            self.touched.append(b)
        for b in wb:
            b.last_write = op
            b.readers = []
            self.touched.append(b)
        self.ops[eng].append(op)
        self.all_ops.append(op)
        return op

    def mm(self, out, lhsT, rhs, start=True, stop=True, **kw):
        return self._rec("pe", lambda e: e.matmul(out.ap, lhsT.ap, rhs.ap, start=start, stop=stop, **kw),
                         [lhsT, rhs] + ([] if start else [out]), [out])

    def tr(self, out, in_, ident):
        return self._rec("pe", lambda e: e.transpose(out.ap, in_.ap, ident.ap), [in_, ident], [out])

    def act(self, out, in_, func, bias=None, scale=None, accum=None):
        reads = [in_]
        kw = {}
        if bias is not None:
            if isinstance(bias, V):
                reads.append(bias)
                kw["bias"] = bias.ap
            else:
                kw["bias"] = bias
        if scale is not None:
            if isinstance(scale, V):
                reads.append(scale)
                kw["scale"] = scale.ap
            else:
                kw["scale"] = scale
        writes = [out]
        if accum is not None:
            kw["accum_out"] = accum.ap
            writes.append(accum)
        return self._rec("act", lambda e: e.activation(out.ap, in_.ap, func, **kw), reads, writes)

    def tt(self, eng, out, a, b, op):
        return self._rec(eng, lambda e: e.tensor_tensor(out.ap, a.ap, b.ap, op), [a, b], [out])

    def ts(self, eng, out, a, s1, s2=None, op0=ALU.mult, op1=None, accum=None):
        reads = [a]
        s1a = s1.ap if isinstance(s1, V) else s1
        s2a = s2.ap if isinstance(s2, V) else s2
        if isinstance(s1, V):
            reads.append(s1)
        if isinstance(s2, V):
            reads.append(s2)
        writes = [out]
        kw = {}
        if op1 is not None:
            kw["op1"] = op1
        if accum is not None:
            kw["accum_out"] = accum.ap
            writes.append(accum)
        return self._rec(eng, lambda e: e.tensor_scalar(out.ap, a.ap, s1a, s2a, op0, **kw), reads, writes)

    def stt(self, eng, out, a, s, b, op0, op1):
        reads = [a, b]
        sa = s.ap if isinstance(s, V) else s
        if isinstance(s, V):
            reads.append(s)
        return self._rec(eng, lambda e: e.scalar_tensor_tensor(out.ap, a.ap, sa, b.ap, op0, op1), reads, [out])

    def copy(self, eng, out, in_):
        if eng == "act":
            return self._rec("act", lambda e: e.copy(out.ap, in_.ap), [in_], [out])
        return self._rec(eng, lambda e: e.tensor_copy(out.ap, in_.ap), [in_], [out])

    def memset(self, eng, out, val):
        return self._rec(eng, lambda e: e.memset(out.ap, val), [], [out])

    def reduce(self, eng, out, in_, op, axis=AX.X):
        return self._rec(eng, lambda e: e.tensor_reduce(out.ap, in_.ap, axis, op), [in_], [out])

    def generic(self, eng, fn, reads, writes):
        return self._rec(eng, fn, reads, writes)

    def dma(self, q, out, in_, sb=None, **kw):
        if sb is None:
            sb = out
        return self._rec(q, lambda e: e.dma_start(out.ap, in_.ap, **kw), [in_], [out], is_dma=True, dbuf=sb)

    def end(self):
        nc = self.nc
        for op in self.all_ops:
            for d in op.deps:
                d.signal = True
        for e in ENGS:
            for op in reversed(self.ops[e]):
                if not op.is_dma:
                    op.signal = True
                    break
        start_tot = {e: self.estream[e].total for e in ENGS}
        assert len(self.dstreams) <= NDSEM, len(self.dstreams)
        for s in self.dstreams:
            s.idx = self.dsem_next % NDSEM
            self.dsem_next += 1
            s.sem = self.dsem_pool[s.idx]
            s.total = self.dsem_base[s.idx]
            s.base = s.total
        for e in ENGS:
            for op in self.ops[e]:
                if op.signal:
                    st = op.stream
                    st.total += st.inc
                    op.count = st.total
        end_tot = {e: self.estream[e].total for e in ENGS}
        nwaits = 0
        with nc.Block() as block:
            def make(ename):
                def body(e):
                    nonlocal nwaits
                    seen = {}
                    for x in ENGS:
                        if x != ename and start_tot[x] > 0:
                            e.wait_ge(self.estream[x].sem, start_tot[x])
                        seen[self.estream[x]] = start_tot[x]
                    for op in self.ops[ename]:
                        need = {}
                        for d in op.deps:
                            st = d.stream
                            if d.count > seen.get(st, 0) and d.count > need.get(st, 0):
                                need[st] = d.count
                        for st, c in need.items():
                            e.wait_ge(st.sem, c)
                            seen[st] = c
                            nwaits += 1
                        ins = op.fn(e)
                        if op.signal:
                            ins.then_inc(op.stream.sem, op.stream.inc)
                    if ename == "sp":
                        for s in self.dstreams:
                            if s.total > s.base:
                                e.wait_ge(s.sem, s.total)
                        for x in ENGS:
                            if x != "sp" and end_tot[x] > start_tot[x]:
                                e.wait_ge(self.estream[x].sem, end_tot[x])
                        st = self.estream["sp"]
                        st.total += 1
                        e.sem_inc(st.sem, 1)
                return body
            for ename in ENGS:
                getattr(block, ENGOBJ[ename])(make(ename))
        for s in self.dstreams:
            self.dsem_base[s.idx] = s.total
        for b in self.touched:
            b.last_write = None
            b.readers = []
            b.dstream = None
        self.tot_ops += len(self.all_ops)
        self.tot_waits += nwaits
        self.nphase += 1
        print('phase', self.pname, 'ops', len(self.all_ops), 'waits', nwaits, 'totals', {e: self.estream[e].total for e in ENGS}, 'ndma_streams', len(self.dstreams), 'sbuf_free', self.nc.sbuf_bytes_remaining, flush=True)
        self.pes.close()
        self.pes = None

    def finish(self):
        self.ges.close()

D = 1024
SEQ = 4096
NT = SEQ // 128
DEPTH = 2
N_IN = 6612
DN_ALPHA = (2 * DEPTH) ** 0.25
NIT = 18
NEG = -1.0e30
TWO_PI = 6.283185307179586
C1 = 6.28125
C2 = TWO_PI - C1

WNAMES = [("ln_in_g", [D]), ("ln_in_b", [D]), ("w_in", [DEPTH, D, N_IN]), ("idx_k_g", [DEPTH, 64]),
          ("gla_wa2", [DEPTH, 16, 256]), ("gla_ba", [DEPTH, 256]), ("gla_norm_g", [DEPTH, 128]),
          ("gm_ln_g", [DEPTH, 512]), ("gm_ln_b", [DEPTH, 512]), ("gm_ws", [DEPTH, 4, 128, 128]),
          ("gm_bs", [DEPTH, 4, 128]), ("w_branch_a", [DEPTH, 512, D]), ("w_branch_b", [DEPTH, 512, D]),
          ("w_branch_c", [DEPTH, 512, D]), ("w_out", [DEPTH, D, D]), ("ln1_g", [DEPTH, D]), ("ln1_b", [DEPTH, D]),
          ("wr_cat", [DEPTH, D, 36]), ("br_cat", [DEPTH, 36]),
          ("w_gate", [DEPTH, 32, D, 256]), ("w_up", [DEPTH, 32, D, 256]), ("w_down", [DEPTH, 32, 256, D]),
          ("ln2_g", [DEPTH, D]), ("ln2_b", [DEPTH, D])]


def host_consts():
    ident = np.eye(128, dtype=np.float32)
    triu = np.triu(np.ones((128, 128), np.float32))
    cbias = np.where(np.arange(128)[None, :] <= np.arange(128)[:, None], 0.0, NEG).astype(np.float32)
    invf = (500000.0 ** (-np.arange(0, 16, 2, dtype=np.float32) / 16)).astype(np.float32)
    invf = np.broadcast_to(invf[None, :], (128, 8)).copy()
    pow2 = np.broadcast_to((0.5 ** np.arange(1, NIT + 1))[None, :], (128, NIT)).astype(np.float32).copy()
    return {"c_ident": ident, "c_triu": triu, "c_cbias": cbias, "c_invf": invf, "c_pow2": pow2}


def brow(v, n):
    return v.re("(o n) -> o n", o=1).bc([128, n])


class K:
    pass


def build(debug=False, stop_after=None, layers=DEPTH):
    nc = bass.Bass("TRN2", target_bir_lowering=False)
    P = Prog(nc)
    k = K()
    k.P = P
    k.debug = debug
    k.skip_abc = stop_after if stop_after in ("M1only", "M2only", "CM1", "BCM1", "AM1") else None
    import os
    k.no_router = bool(os.environ.get("NO_ROUTER"))
    k.stop_m1 = (stop_after == "M1")
    if stop_after == "M1":
        stop_after = "M"
    dk = "ExternalOutput" if debug else "Internal"
    k.x = P.dram("x", [SEQ, D], F32, kind="ExternalInput")
    k.pos = P.dram("pos", [128, NT], I32, kind="ExternalInput")
    k.w = {}
    for n, shp in WNAMES:
        k.w[n] = P.dram(n, shp, F32, kind="ExternalInput")
    k.c = {}
    for n, a in host_consts().items():
        k.c[n] = P.dram(n, list(a.shape), F32, kind="ExternalInput")
    k.out = P.dram("out", [SEQ, D], F32, kind="ExternalOutput")
    k.hres = P.dram("hres", [SEQ, D], F32, kind=dk)
    k.YA = P.dram("YA", [NT, 128, 4, 128], BF16, kind=dk)
    k.YB = P.dram("YB", [NT, 128, 4, 128], BF16, kind=dk)
    k.YC = P.dram("YC", [NT, 128, 4, 128], BF16, kind=dk)
    k.hres_b = [Buf("hres%d" % t) for t in range(NT)]
    k.HM = P.dram("HM", [NT, 128, D], BF16, kind=dk)
    k.HM_b = [Buf("HM%d" % t) for t in range(NT)]
    k.Y_b = {n: [Buf("%s%d" % (n, t)) for t in range(NT)] for n in ("YA", "YB", "YC")}

    hT_all = P.sbuf("hT", [128, 8, SEQ], BF16, glob=True)
    k.hT_b = [Buf("hT%d" % t) for t in range(NT)]
    k.hT_all = hT_all
    k.ident = P.sbuf("ident", [128, 128], F32, glob=True)
    k.identb = P.sbuf("identb", [128, 128], BF16, glob=True)
    k.triu = P.sbuf("triu", [128, 128], F32, glob=True)
    k.cbias = P.sbuf("cbias", [128, 128], F32, glob=True)
    k.ones = P.sbuf("ones", [128, 128], F32, glob=True)
    k.onesb = P.sbuf("onesb", [128, 128], BF16, glob=True)
    k.eps5 = P.sbuf("eps5", [128, 1], F32, glob=True)
    k.eps6 = P.sbuf("eps6", [128, 1], F32, glob=True)
    k.COS = P.sbuf("COS", [128, NT, 8], F32, glob=True)
    k.SIN = P.sbuf("SIN", [128, NT, 8], F32, glob=True)
    k.COMB = P.sbuf("COMB", [128, NT, 32], F32, glob=True)
    k.pow2 = P.sbuf("pow2", [128, NIT], F32, glob=True)

    phase0(k)
    if stop_after == "p0":
        return fin(k)
    for l in range(layers):
        if k.skip_abc:
            if k.skip_abc == "M1only":
                phaseM1(k, l)
            if k.skip_abc == "M2only":
                phaseM2(k, l)
            if k.skip_abc == "CM1":
                phaseC(k, l); phaseM1(k, l)
            if k.skip_abc == "BCM1":
                phaseB(k, l); phaseC(k, l); phaseM1(k, l)
            if k.skip_abc == "AM1":
                phaseA(k, l); phaseM1(k, l)
            return fin(k)
        phaseA(k, l)
        if stop_after == "A":
            return fin(k)
        phaseB(k, l)
        if stop_after == "B":
            return fin(k)
        phaseC(k, l)
        if stop_after == "C":
            return fin(k)
        phaseM(k, l)
        if stop_after == "M":
            return fin(k)
        phaseE(k, l, last=(l == layers - 1))
    return fin(k)


def fin(k):
    k.P.finish()
    return k.P.nc, k.P


def hT(k, t):
    return V(k.hT_all.ap[:, :, t * 128:(t + 1) * 128], k.hT_b[t])


def hT_span(k, dc, t0, nt):
    return V(k.hT_all.ap[:, dc, t0 * 128:(t0 + nt) * 128], tuple(k.hT_b[t0:t0 + nt]))


def hres_tile(k, t):
    return V(k.hres.ap[t * 128:(t + 1) * 128, :], k.hres_b[t])


def load_w(P, dst, src, q="pool", engs=("pool",), nst=3):
    n = src.ap.shape[1]
    import os
    if not USE_SWDGE:
        if not hasattr(P, "_stage") or P._stage_phase != P.nphase:
            P._stage = P.sbuf("wstage", [128, nst, 1024], F32, nslots=nst)
            P._stage_phase = P.nphase
            P._stage_i = 0
        nch = src.ap.shape[0] // 128
        ns = len(P._stage)
        if n < 1024 and 1024 % n == 0 and nch % (1024 // n) == 0 and len(dst.ap.shape) == 3:
            G = 1024 // n
            for c in range(0, nch, G):
                st = P._stage[P._stage_i % ns]
                ce = engs[P._stage_i % len(engs)]
                P._stage_i += 1
                P.dma("sp", st.re("p (g n) -> p g n", g=G), src[c * 128:(c + G) * 128, :].re("(g p) n -> p g n", p=128), sb=st)
                P.copy(ce, dst[:, c:c + G, :], st.re("p (g n) -> p g n", g=G))
            return
        for c0 in range(0, n, 1024):
            for c in range(nch):
                c1 = min(n, c0 + 1024)
                st = P._stage[P._stage_i % ns]
                ce = engs[P._stage_i % len(engs)]
                P._stage_i += 1
                P.dma("sp", st[:, 0:c1 - c0], src[c * 128:(c + 1) * 128, c0:c1], sb=st)
                P.copy(ce, dst[:, c, c0:c1], st[:, 0:c1 - c0])
        return
    for c0 in range(0, n, 1024):
        c1 = min(n, c0 + 1024)
        P.dma(q, dst[:, :, c0:c1], src[:, c0:c1].re("(c p) n -> p c n", p=128), sb=dst)


class LN:
    def __init__(self, k, name, Dn, eps_t, nb=2):
        P = k.P
        self.k = k
        self.Dn = Dn
        self.nch = max(1, Dn // 512)
        self.cw = min(Dn, 512)
        self.st = P.sbuf(name + "_st", [128, nb, self.nch * 6], F32, nslots=nb)
        self.mv = P.sbuf(name + "_mv", [128, nb, 2], F32, nslots=nb)
        self.lv = P.sbuf(name + "_lv", [128, nb, 1], F32, nslots=nb)
        self.rs = P.sbuf(name + "_rs", [128, nb, 1], F32, nslots=nb)
        self.eps = eps_t
        self.nb = nb
        self.i = 0

    def __call__(self, r, y, gbc=None, bbc=None, eng_aff="pool", eng_bias=None):
        self.part1(r, y)
        self.part2(y, gbc, bbc, eng_aff, eng_bias)

    def part2(self, y, gbc=None, bbc=None, eng_aff="pool", eng_bias=None):
        P = self.k.P
        if gbc is not None:
            P.tt(eng_aff, y, y, gbc, ALU.mult)
        if bbc is not None:
            P.tt(eng_bias or eng_aff, y, y, bbc, ALU.add)

    def part1(self, r, y):
        P = self.k.P
        j = self.i % self.nb
        self.i += 1
        st, mv, lv, rs = self.st[j], self.mv[j], self.lv[j], self.rs[j]
        for c in range(self.nch):
            P.generic("dve", lambda e, c=c: e.bn_stats(st.ap[:, c * 6:(c + 1) * 6], r.ap[:, c * self.cw:(c + 1) * self.cw]),
                      [r], [st])
        P.generic("dve", lambda e: e.bn_aggr(mv.ap, st.ap), [st], [mv])
        P.act(lv, mv[:, 1:2], AF.Ln, bias=self.eps, scale=1.0)
        P.act(rs, lv, AF.Exp, scale=-0.5)
        P.ts("dve", y, r, mv[:, 0:1], rs, op0=ALU.subtract, op1=ALU.mult)


def phase0(k):
    P = k.P
    P.begin("p0")
    for name, t in (("c_ident", k.ident), ("c_triu", k.triu), ("c_cbias", k.cbias), ("c_pow2", k.pow2)):
        P.dma("sp", t, k.c[name])
    P.copy("dve", k.identb, k.ident)
    P.memset("pool", k.ones, 1.0)
    P.memset("pool", k.onesb, 1.0)
    P.memset("pool", k.eps5, 1e-5)
    P.memset("pool", k.eps6, 1e-6)
    posi = P.sbuf("posi", [128, NT], I32)
    posf = P.sbuf("posf", [128, NT], F32)
    invf = P.sbuf("invf", [128, 8], F32)
    ang = P.sbuf("ang", [128, NT, 8], F32)
    a2 = P.sbuf("a2", [128, NT, 8], F32)
    kf = P.sbuf("kf", [128, NT, 8], F32)
    ki = P.sbuf("ki", [128, NT, 8], I32)
    P.dma("sp", posi, k.pos)
    P.dma("sp", invf, k.c["c_invf"])
    P.copy("dve", posf, posi)
    P.tt("dve", ang, posf.un(2).bc([128, NT, 8]), invf.un(1).bc([128, NT, 8]), ALU.mult)
    for tab, shift in ((k.SIN, 0.0), (k.COS, np.pi / 2)):
        if shift != 0.0:
            P.ts("dve", a2, ang, float(shift), None, op0=ALU.add)
            src = a2
        else:
            src = ang
        P.ts("dve", kf, src, float(1.0 / TWO_PI), None, op0=ALU.mult)
        P.copy("dve", ki, kf)
        P.copy("dve", kf, ki)
        P.stt("dve", a2, kf, float(-C1), src, ALU.mult, ALU.add)
        P.stt("dve", a2, kf, float(-C2), a2, ALU.mult, ALU.add)
        P.ts("dve", a2, a2, float(np.pi), float(-np.pi), op0=ALU.min, op1=ALU.max)
        P.act(tab, a2, AF.Sin)
    gbc = P.sbuf("gin", [128, D], F32)
    bbc = P.sbuf("bin", [128, D], F32)
    P.dma("sp", gbc, brow(k.w["ln_in_g"], D))
    P.dma("sp", bbc, brow(k.w["ln_in_b"], D))
    xt = P.sbuf("xt", [128, 2, D], F32, nslots=2)
    yt = P.sbuf("yt", [128, 2, D], F32, nslots=2)
    pT = P.psum("pT", [128, 2, 8, 128], F32, nslots=2)
    ln = LN(k, "ln0", D, k.eps5)
    P.dma("sp", xt[0], k.x[0:128, :])
    P.dma("sp", xt[1], k.x[128:256, :])
    ln.part1(xt[0], yt[0])
    for t in range(NT):
        j = t % 2
        if t + 1 < NT:
            ln.part1(xt[(t + 1) % 2], yt[(t + 1) % 2])
        if t + 2 < NT:
            P.dma("sp", xt[j], k.x[(t + 2) * 128:(t + 3) * 128, :])
        ln.part2(yt[j], gbc, bbc, "pool", "dve")
        P.dma("sp", hres_tile(k, t), yt[j], sb=yt[j])
        for c in range(8):
            P.tr(pT[j][:, c, :], yt[j][:, c * 128:(c + 1) * 128], k.ident)
        P.copy("act", hT(k, t), pT[j])
    P.end()


def phaseA(k, l):
    P = k.P
    P.begin("A%d" % l)
    w_in = k.w["w_in"][l]
    WA = P.sbuf("WA", [128, 8, 964], BF16)
    load_w(P, WA, w_in[:, 0:964], engs=("pool", "act", "dve"))
    gik = P.sbuf("gik", [128, 64], F32)
    P.dma("sp", gik, brow(k.w["idx_k_g"][l], 64))
    ident4 = P.sbuf("ident4", [128, 4, 128], BF16)
    P.copy("pool", ident4, k.identb.un(1).bc([128, 4, 128]))
    id4 = ident4.re("p a b -> p (a b)")
    kT = P.sbuf("kT", [64, SEQ], BF16)
    kiT = P.sbuf("kiT", [64, SEQ], BF16)
    kT_b = [Buf("kT%d" % t) for t in range(NT)]
    kiT_b = [Buf("kiT%d" % t) for t in range(NT)]
    V1 = P.sbuf("V1", [128, NT, 65], BF16)
    V1_b = [Buf("V1_%d" % t) for t in range(NT)]
    P.memset("pool", V1.wb(tuple(V1_b)), 1.0)
    X = P.sbuf("X", [128, 2, 964], F32, nslots=2)
    Xr = P.sbuf("Xr", [128, 2, 960], BF16, nslots=2)
    rt = P.sbuf("rt", [128, 2, 4, 15, 8], F32, nslots=2)
    wsc = P.sbuf("wsc", [128, 2, 4], F32, nslots=2)
    lns = P.sbuf("lns", [128, 2, 8], F32, nslots=2)
    j64 = P.sbuf("j64", [128, 64], F32)
    qTb = P.sbuf("qTb", [64, 3, 1024], BF16, nslots=3)
    qiTb = P.sbuf("qiTb", [64, 2, 512], BF16, nslots=2)
    S = P.sbuf("S", [128, 2, SEQ], F32, nslots=2)
    rl = P.sbuf("rl", [128, 3, 512], F32, nslots=3)
    mb = P.sbuf("mb", [128, 2, SEQ], BF16, nslots=2)
    bs = P.sbuf("bs", [128, 2, 8], F32, nslots=2)
    cn = P.sbuf("cn", [128, 2, NIT], F32, nslots=2)
    halves = P.sbuf("halves", [128, 2, NIT], F32, nslots=2)
    E = P.sbuf("E", [128, 2, 1024], BF16, nslots=2)
    rec = P.sbuf("rec", [128, 2, 8], F32, nslots=2)
    ya = P.sbuf("ya", [128, 2, 512], BF16, nslots=2)
    yaT = P.sbuf("yaT", [128, 2, 512], BF16, nslots=2)
    pAS = P.psum("pAS", [128, 2, 512], F32, nslots=2)
    pT = P.psum("pT", [128, 16, 128], BF16)
    pL = P.psum("pL", [128, 2, 512], F32, nslots=2)
    pO = P.psum("pO", [128, 2, 512], F32, nslots=2)
    cnt = [0]

    def front_units(qb):
        j = qb % 2
        nk = (qb + 1) * 128
        Xj, Xrj = X[j], Xr[j]
        X3 = Xj[:, 0:960].re("p (s d) -> p s d", d=64)
        R3 = Xrj.re("p (s d) -> p s d", d=64)
        Sj = S[j]
        units = []

        def u_proj():
            for (ps, c0, c1) in ((pAS[0], 0, 512), (pAS[1], 512, 964)):
                for dc in range(8):
                    P.mm(ps[:, 0:c1 - c0], hT(k, qb)[:, dc, :], WA[:, dc, c0:c1], start=(dc == 0), stop=(dc == 7))
                P.copy("act", Xj[:, c0:c1], ps[:, 0:c1 - c0])
        units.append(u_proj)

        def u_ln_rope():
            xs = Xj[:, 896:960]
            ls = lns[j]
            s1, s2, m2, bb, lv, rs, nm, nmr = (ls[:, i:i + 1] for i in range(8))
            P.memset("pool", ls[:, 0:2], 0.0)
            P.act(j64, xs, AF.Identity, accum=s1)
            P.act(j64, xs, AF.Square, accum=s2)
            P.act(m2, s1, AF.Square, scale=1.0 / 64)
            P.act(bb, m2, AF.Identity, bias=k.eps5, scale=-1.0)
            P.act(lv, s2, AF.Ln, bias=bb, scale=1.0 / 64)
            P.act(rs, lv, AF.Exp, scale=-0.5)
            P.act(nm, s1, AF.Identity, scale=-1.0 / 64)
            P.act(nmr, nm, AF.Identity, scale=rs)
            P.act(xs, xs, AF.Identity, bias=nmr, scale=rs)
            P.tt("pool", xs, xs, gik, ALU.mult)
            P._rec("pool", lambda e: e.tensor_copy(V1.ap[:, qb, 0:64], Xj.ap[:, 576:640]), [Xj], [V1_b[qb]])
            P.ts("pool", wsc[j], Xj[:, 960:964], 0.0625, None, op0=ALU.mult)
            cb = k.COS[:, qb, :].un(1).bc([128, 15, 8])
            sb_ = k.SIN[:, qb, :].un(1).bc([128, 15, 8])
            t1, t2, t3, t4 = (rt[j][:, i] for i in range(4))
            P.copy("pool", R3[:, :, 16:64], X3[:, :, 16:64])
            P.tt("pool", t1, X3[:, :, 0:8], cb, ALU.mult)
            P.tt("pool", t2, X3[:, :, 8:16], sb_, ALU.mult)
            P.tt("pool", t3, X3[:, :, 0:8], sb_, ALU.mult)
            P.tt("pool", t4, X3[:, :, 8:16], cb, ALU.mult)
            P.tt("pool", R3[:, :, 0:8], t1, t2, ALU.subtract)
            P.tt("pool", R3[:, :, 8:16], t3, t4, ALU.add)
        units.append(u_ln_rope)

        def u_tr():
            slots = list(range(0, 9)) + list(range(10, 15))
            for i, s in enumerate(slots):
                P.tr(pT[0:64, i, :], R3[:, s, :], k.identb)
            P.copy("act", qTb[qb % 3], pT[0:64, 0:8, :].re("p a b -> p (a b)"))
            P.copy("act", V(kT.ap[:, qb * 128:(qb + 1) * 128], kT_b[qb]), pT[0:64, 8, :])
            P.copy("act", qiTb[j], pT[0:64, 9:13, :].re("p a b -> p (a b)"))
            P.copy("act", V(kiT.ap[:, qb * 128:(qb + 1) * 128], kiT_b[qb]), pT[0:64, 13, :])
        units.append(u_tr)

        nch = (nk + 511) // 512
        for ch in range(nch):
            for h in range(4):
                def u_s(ch=ch, h=h):
                    kw = min(512, nk - ch * 512)
                    tb = tuple(kiT_b[ch * 4:ch * 4 + kw // 128])
                    sl_ = Sj[:, ch * 512:ch * 512 + kw]
                    ps = pAS[cnt[0] % 2]
                    r_ = rl[cnt[0] % 3]
                    cnt[0] += 1
                    P.mm(ps[:, 0:kw], qiTb[j][:, h * 128:(h + 1) * 128], V(kiT.ap[:, ch * 512:ch * 512 + kw], tb))
                    P.act(r_[:, 0:kw], ps[:, 0:kw], AF.Relu)
                    wb_ = wsc[j][:, h:h + 1].bc([128, kw])
                    if h == 0:
                        P.tt("pool", sl_, r_[:, 0:kw], wb_, ALU.mult)
                    else:
                        P.tt("pool", r_[:, 0:kw], r_[:, 0:kw], wb_, ALU.mult)
                        P.tt("pool", sl_, sl_, r_[:, 0:kw], ALU.add)
                    if ch == nch - 1 and h == 3:
                        P.tt("pool", Sj[:, qb * 128:nk], Sj[:, qb * 128:nk], k.cbias, ALU.add)
                units.append(u_s)
        return units

    def thr(qb):
        j = qb % 2
        nk = (qb + 1) * 128
        Sj, mk = S[j], mb[j]
        b_ = bs[j]
        lo, hi, rng, tr_, d_, th = (b_[:, i:i + 1] for i in range(6))
        if qb >= 2:
            nd = qb * 128
            P.reduce("dve", hi, Sj[:, 0:nd], ALU.max)
            P.reduce("dve", lo, Sj[:, 0:nd], ALU.min)
            P.tt("dve", rng, hi, lo, ALU.subtract)
            P.ts("dve", halves[j], k.pow2, rng, None, op0=ALU.mult)
            P.tt("dve", tr_, lo, halves[j][:, 0:1], ALU.add)
            P.memset("dve", cn[j], 0.0)
            for it in range(NIT):
                c_ = cn[j][:, it:it + 1]
                h_ = halves[j][:, it:it + 1]
                P.ts("dve", mk[:, 0:nk], Sj[:, 0:nk], tr_, 0.0, op0=ALU.is_ge, op1=ALU.add, accum=c_)
                P.ts("dve", d_, c_, 255.5, 0.5, op0=ALU.is_ge, op1=ALU.subtract)
                P.stt("dve", tr_, d_, h_, tr_, ALU.mult, ALU.add)
            P.stt("dve", th, halves[j][:, NIT - 1:NIT], -0.5, tr_, ALU.mult, ALU.add)
        else:
            P.memset("dve", th, -1.0e29)
        P.ts("dve", mk[:, 0:nk], Sj[:, 0:nk], th, -30000.0, op0=ALU.is_lt, op1=ALU.mult)

    def att_units(qb):
        j = qb % 2
        mk = mb[j]
        units = []

        def pv(kc):
            v1 = V(V1.ap[:, kc, :], V1_b[kc])
            for h in range(8):
                P.mm(pO[h // 4][:, (h % 4) * 65:(h % 4) * 65 + 65], E[kc % 2][:, h * 128:(h + 1) * 128], v1,
                     start=(kc == 0 and h % 4 == 0), stop=(kc == qb and h % 4 == 3))

        for kc in range(qb + 1):
            def u(kc=kc):
                kTv = V(kT.ap[:, kc * 128:(kc + 1) * 128], kT_b[kc])
                for hh in range(2):
                    P.mm(pL[hh], kTv, qTb[qb % 3][:, hh * 512:(hh + 1) * 512], start=True, stop=False)
                    P.mm(pL[hh], mk[:, kc * 128:(kc + 1) * 128], id4, start=False, stop=True)
                    P.act(E[kc % 2][:, hh * 512:(hh + 1) * 512], pL[hh], AF.Exp, scale=0.125)
                if kc > 0:
                    pv(kc - 1)
                if kc == qb:
                    pv(kc)
            units.append(u)
        return units

    def norm(qb):
        j = qb % 2
        for hh in range(2):
            o3 = pO[hh][:, 0:260].re("p (h d) -> p h d", d=65)
            rc = rec[j][:, hh * 4:(hh + 1) * 4]
            P.generic("dve", lambda e, rc=rc, o3=o3: e.reciprocal(rc.ap, o3.ap[:, :, 64]), [o3], [rc])
            P.tt("dve", ya[j][:, hh * 256:(hh + 1) * 256].re("p (h d) -> p h d", d=64), o3[:, :, 0:64],
                 rc.un(2).bc([128, 4, 64]), ALU.mult)
        for c in range(4):
            P.tr(pT[:, c, :], ya[j][:, c * 128:(c + 1) * 128], k.identb)
        P.copy("act", yaT[j], pT[:, 0:4, :].re("p a b -> p (a b)"))
        P.dma("sp", V(k.YA.ap[qb].rearrange("p a b -> p (a b)"), k.Y_b["YA"][qb]), yaT[j], sb=yaT[j])

    def run_merged(a, b):
        import os
        if os.environ.get("A_INTERLEAVE") == "0":
            for f in b:
                f()
            for f in a:
                f()
            return
        ia = ib = 0
        while ia < len(a) or ib < len(b):
            if ib >= len(b) or (ia < len(a) and ia * len(b) <= ib * len(a)):
                a[ia]()
                ia += 1
            else:
                b[ib]()
                ib += 1

    run_merged(front_units(0), [])
    thr(0)
    run_merged(front_units(1), [])
    for i in range(NT):
        if i + 1 < NT:
            thr(i + 1)
        run_merged(front_units(i + 2) if i + 2 < NT else [], att_units(i))
        norm(i)
    P.end()


def phaseB(k, l):
    P = k.P
    P.begin("B%d" % l)
    w_in = k.w["w_in"][l]
    WB = P.sbuf("WB", [128, 8, 1552], BF16)
    load_w(P, WB[:, :, 0:1024], w_in[:, 964:1988], engs=("pool", "act", "dve"), nst=6)
    load_w(P, WB[:, :, 1024:1536], w_in[:, 2004:2516], engs=("pool", "act", "dve"))
    load_w(P, WB[:, :, 1536:1552], w_in[:, 1988:2004], engs=("pool", "act", "dve"))
    wa2 = P.sbuf("wa2", [16, 256], BF16)
    wa2f = P.sbuf("wa2f", [16, 256], F32)
    P.dma("sp", wa2f, k.w["gla_wa2"][l])
    P.copy("pool", wa2, wa2f)
    ba = P.sbuf("ba", [1, 256], BF16)
    baf = P.sbuf("baf", [1, 256], F32)
    P.dma("sp", baf, k.w["gla_ba"][l].re("(o n) -> o n", o=1))
    P.copy("pool", ba, baf)
    gng = P.sbuf("gng", [128, 128], F32)
    P.dma("sp", gng, brow(k.w["gla_norm_g"][l], 128))
    triub = P.sbuf("triub", [128, 128], BF16)
    P.copy("dve", triub, k.triu)
    St = P.sbuf("St", [64, 4, 128], F32)
    Sb = P.sbuf("Sb", [64, 2, 4, 128], BF16, nslots=2)
    glT = P.sbuf("glT", [16, 2, 128], BF16, nslots=2)
    e1 = P.sbuf("e1", [128, 2, 256], F32, nslots=2)
    lg = P.sbuf("lg", [128, 2, 256], F32, nslots=2)
    bsb = P.sbuf("bsb", [128, 2, 256], F32, nslots=2)
    dd = P.sbuf("dd", [128, 2, 256], F32, nslots=2)
    eq = P.sbuf("eq", [128, 2, 256], F32, nslots=2)
    ek = P.sbuf("ek", [128, 2, 256], F32, nslots=2)
    ekl = P.sbuf("ekl", [128, 2, 256], F32, nslots=2)
    qt = P.sbuf("qt", [128, 2, 256], BF16, nslots=2)
    kt = P.sbuf("kt", [128, 2, 256], BF16, nslots=2)
    kh = P.sbuf("kh", [128, 2, 256], BF16, nslots=2)
    vb = P.sbuf("vb", [128, 2, 512], BF16, nslots=2)
    dec = P.sbuf("dec", [64, 2, 4], F32, nslots=2)
    qkT = P.sbuf("qkT", [64, 2, 1024], BF16, nslots=2)
    att = P.sbuf("att", [128, 2, 512], BF16, nslots=2)
    ss = P.sbuf("ss", [128, 2, 4], F32, nslots=2)
    sl = P.sbuf("sl", [128, 2, 4], F32, nslots=2)
    ri = P.sbuf("ri", [128, 2, 4], F32, nslots=2)
    sq = P.sbuf("sq", [128, 128], BF16)
    yb = P.sbuf("yb", [128, 2, 512], F32, nslots=2)
    sr = P.sbuf("sr", [128, 2, 512], F32, nslots=2)
    yb2 = P.sbuf("yb2", [128, 2, 512], BF16, nslots=2)
    ybT = P.sbuf("ybT", [128, 2, 512], BF16, nslots=2)
    pQA = P.psum("pQA", [128, 512], F32)
    pV = P.psum("pV", [128, 512], F32)
    pR = P.psum("pR", [128, 512], F32)
    pZG = P.psum("pZG", [128, 512], F32)
    pCS = P.psum("pCS", [128, 512], F32)
    pTr = P.psum("pTr", [128, 8, 128], BF16)
    pOo = P.psum("pOo", [128, 512], F32)
    pX = P.psum("pX", [128, 512], F32)

    def front_units(t):
        j = t % 2
        h_ = hT(k, t)

        def u1():
            for dc in range(8):
                P.mm(pQA, h_[:, dc, :], WB[:, dc, 0:512], start=(dc == 0), stop=(dc == 7))
            for dc in range(8):
                P.mm(pV, h_[:, dc, :], WB[:, dc, 512:1024], start=(dc == 0), stop=(dc == 7))
            for dc in range(8):
                P.mm(pR, h_[:, dc, :], WB[:, dc, 1024:1536], start=(dc == 0), stop=(dc == 7))
            for dc in range(8):
                P.mm(pZG[0:16, 256:384], WB[:, dc, 1536:1552], h_[:, dc, :], start=(dc == 0), stop=(dc == 7))
            P.copy("act", glT[j], pZG[0:16, 256:384])

        def u2():
            P.mm(pZG[:, 0:256], glT[j], wa2, start=True, stop=False)
            P.mm(pZG[:, 0:256], k.onesb[0:1, :], ba, start=False, stop=True)
            P.act(e1[j], pZG[:, 0:256], AF.Exp, scale=-1.0)
            P.act(lg[j], e1[j], AF.Ln, bias=k.ones[:, 0:1], scale=1.0)

        def u3():
            P.mm(pCS[:, 0:256], k.triu, lg[j])
            P.mm(pCS[:, 256:512], k.ones, lg[j])
            for h in range(4):
                P.mm(pZG[0:64, 384 + h:385 + h], lg[j][:, h * 64:(h + 1) * 64], k.ones[:, 0:1])
            P.act(dec[j], pZG[0:64, 384:388], AF.Exp, scale=-1.0 / 16)
            P.act(eq[j], pCS[:, 0:256], AF.Exp, scale=-1.0 / 16)
            P.act(ek[j], pCS[:, 0:256], AF.Exp, scale=1.0 / 16)
            P.copy("act", bsb[j], pCS[:, 0:256])
            P.tt("dve", dd[j], pCS[:, 256:512], bsb[j], ALU.subtract)
            P.act(ekl[j], dd[j], AF.Exp, scale=-1.0 / 16)
            P.stt("dve", qt[j], pQA[:, 0:256], 0.125, eq[j], ALU.mult, ALU.mult)
            P.tt("dve", kt[j], pQA[:, 256:512], ek[j], ALU.mult)
            P.tt("dve", kh[j], pQA[:, 256:512], ekl[j], ALU.mult)
            P.copy("act", vb[j], pV)
            P.act(sr[j], pR, AF.Silu)
        return [u1, u2, u3]

    def back_units(t):
        j = t % 2

        def v1():
            for h in range(4):
                P.tr(pTr[0:64, h, :], qt[j][:, h * 64:(h + 1) * 64], k.identb)
                P.tr(pTr[0:64, 4 + h, :], kt[j][:, h * 64:(h + 1) * 64], k.identb)
            P.copy("act", qkT[j], pTr[0:64, :, :].re("p a b -> p (a b)"))

        def v2():
            for h in range(4):
                P.mm(pX[:, h * 128:(h + 1) * 128], qkT[j][:, (4 + h) * 128:(5 + h) * 128], qkT[j][:, h * 128:(h + 1) * 128])
            P.tt("dve", att[j].re("p (h t) -> p h t", h=4), pX.re("p (h t) -> p h t", h=4),
                 triub.un(1).bc([128, 4, 128]), ALU.mult)

        def v3():
            Sprev = Sb[(t + 1) % 2]
            for h in range(4):
                P.mm(pOo[:, h * 128:(h + 1) * 128], att[j][:, h * 128:(h + 1) * 128], vb[j][:, h * 128:(h + 1) * 128],
                     start=True, stop=(t == 0))
                if t > 0:
                    P.mm(pOo[:, h * 128:(h + 1) * 128], qkT[j][:, h * 128:(h + 1) * 128], Sprev[:, h, :],
                         start=False, stop=True)
            if t < NT - 1:
                pSt = pX[0:64, :]
                for h in range(4):
                    P.mm(pSt[:, h * 128:(h + 1) * 128], kh[j][:, h * 64:(h + 1) * 64], vb[j][:, h * 128:(h + 1) * 128])
                if t == 0:
                    P.copy("dve", St, pSt.re("p (h v) -> p h v", h=4))
                else:
                    for h in range(4):
                        P.stt("dve", St[:, h, :], St[:, h, :], dec[j][:, h:h + 1], pSt[:, h * 128:(h + 1) * 128],
                              ALU.mult, ALU.add)
                P.copy("act", Sb[j], St)

        def v4():
            P.memset("pool", ss[j], 0.0)
            for h in range(4):
                P.act(sq, pOo[:, h * 128:(h + 1) * 128], AF.Square, accum=ss[j][:, h:h + 1])
            P.act(sl[j], ss[j], AF.Ln, bias=k.eps6, scale=1.0 / 128)
            P.act(ri[j], sl[j], AF.Exp, scale=-0.5)
            P.tt("dve", yb[j].re("p (h v) -> p h v", h=4), pOo.re("p (h v) -> p h v", h=4),
                 ri[j].un(2).bc([128, 4, 128]), ALU.mult)
            P.tt("pool", yb[j].re("p (h v) -> p h v", h=4), yb[j].re("p (h v) -> p h v", h=4),
                 gng.un(1).bc([128, 4, 128]), ALU.mult)
            P.tt("pool", yb2[j], yb[j], sr[j], ALU.mult)

        def v5():
            for c in range(4):
                P.tr(pTr[:, c, :], yb2[j][:, c * 128:(c + 1) * 128], k.identb)
            P.copy("act", ybT[j], pTr[:, 0:4, :].re("p a b -> p (a b)"))
            P.dma("sp", V(k.YB.ap[t].rearrange("p a b -> p (a b)"), k.Y_b["YB"][t]), ybT[j], sb=ybT[j])
        return [v1, v2, v3, v4, v5]

    def run_merged(a, b):
        ia = ib = 0
        while ia < len(a) or ib < len(b):
            if ib >= len(b) or (ia < len(a) and ia * len(b) <= ib * len(a)):
                a[ia]()
                ia += 1
            else:
                b[ib]()
                ib += 1

    run_merged(front_units(0), [])
    for t in range(NT):
        run_merged(front_units(t + 1) if t + 1 < NT else [], back_units(t))
    P.end()


def phaseC(k, l):
    P = k.P
    P.begin("C%d" % l)
    w_in = k.w["w_in"][l]
    WC = P.sbuf("WC", [128, 8, 1024], BF16)
    load_w(P, WC, w_in[:, 2516:3540], engs=("pool", "act", "dve"), nst=6)
    gg = P.sbuf("gg", [128, 512], F32)
    gb = P.sbuf("gb", [128, 512], F32)
    P.dma("sp", gg, brow(k.w["gm_ln_g"][l], 512))
    P.dma("sp", gb, brow(k.w["gm_ln_b"][l], 512))
    wsf = P.sbuf("wsf", [128, 4, 128], F32)
    P.dma("sp", wsf, k.w["gm_ws"][l].re("g t s -> t g s"))
    bsT = P.sbuf("bsT", [128, 4], F32)
    P.dma("sp", bsT, k.w["gm_bs"][l].re("g t -> t g"), allow_slow_non_contiguous=True)
    WsT = P.sbuf("WsT", [128, 4, 128], BF16)
    pW = P.psum("pW", [128, 4, 128], F32)
    for g in range(4):
        P.tr(pW[:, g, :], wsf[:, g, :], k.ident)
    P.tt("dve", WsT, pW, k.triu.un(1).bc([128, 4, 128]), ALU.mult)
    pU = P.psum("pU", [128, 2, 512], F32, nslots=2)
    pVv = P.psum("pVv", [128, 2, 512], F32, nslots=2)
    pM = P.psum("pM", [128, 512], F32)
    pTr = P.psum("pTr", [128, 4, 128], BF16)
    u = P.sbuf("u", [128, 2, 512], F32, nslots=2)
    vv = P.sbuf("vv", [128, 2, 512], F32, nslots=2)
    vn = P.sbuf("vn", [128, 2, 512], F32, nslots=2)
    vnb = P.sbuf("vnb", [128, 2, 512], BF16, nslots=2)
    mx = P.sbuf("mx", [128, 2, 512], F32, nslots=2)
    yc = P.sbuf("yc", [128, 2, 512], BF16, nslots=2)
    ycT = P.sbuf("ycT", [128, 2, 512], BF16, nslots=2)
    ln = LN(k, "lnc", 512, k.eps5)

    def s1(t):
        j = t % 2
        h_ = hT(k, t)
        for dc in range(8):
            P.mm(pU[j], h_[:, dc, :], WC[:, dc, 0:512], start=(dc == 0), stop=(dc == 7))
        for dc in range(8):
            P.mm(pVv[j], h_[:, dc, :], WC[:, dc, 512:1024], start=(dc == 0), stop=(dc == 7))
        P.act(u[j], pU[j], AF.Gelu_apprx_tanh)
        P.act(vv[j], pVv[j], AF.Gelu_apprx_tanh)
        ln(vv[j], vn[j], gg, None)
        P.tt("pool", vnb[j], vn[j], gb, ALU.add)

    def s2(t):
        j = t % 2
        for g in range(4):
            P.mm(pM[:, g * 128:(g + 1) * 128], WsT[:, g, :], vnb[j][:, g * 128:(g + 1) * 128])
        P.tt("dve", mx[j].re("p (g d) -> p g d", g=4), pM.re("p (g d) -> p g d", g=4),
             bsT.un(2).bc([128, 4, 128]), ALU.add)
        P.tt("pool", yc[j], mx[j], u[j], ALU.mult)

    def s3(t):
        j = t % 2
        for c in range(4):
            P.tr(pTr[:, c, :], yc[j][:, c * 128:(c + 1) * 128], k.identb)
        P.copy("act", ycT[j], pTr.re("p a b -> p (a b)"))
        P.dma("sp", V(k.YC.ap[t].rearrange("p a b -> p (a b)"), k.Y_b["YC"][t]), ycT[j], sb=ycT[j])

    s1(0)
    s1(1)
    s2(0)
    for t in range(NT):
        if t + 2 < NT:
            s1(t + 2)
        if t + 1 < NT:
            s2(t + 1)
        s3(t)
    P.end()


def phaseM(k, l):
    phaseM1(k, l)
    if getattr(k, "stop_m1", False):
        return
    phaseM2(k, l)


def phaseM1(k, l):
    P = k.P
    P.begin("M1_%d" % l)
    w_in = k.w["w_in"][l]
    WG = P.sbuf("WG", [128, 8, 3072], BF16)
    load_w(P, WG, w_in[:, 3540:6612], engs=("pool", "act", "dve"), nst=5)
    WBR = P.sbuf("WBR", [128, 3, 4, 1024], BF16)
    for i, n in enumerate(("w_branch_a", "w_branch_b", "w_branch_c")):
        load_w(P, WBR[:, i], k.w[n][l], engs=("pool", "act", "dve"))
    yT = {n: P.sbuf("m" + n, [128, 2, 512], BF16, nslots=2) for n in ("YA", "YB", "YC")}
    sg = P.sbuf("sg", [128, 2, 512], F32, nslots=2)
    tmp = P.sbuf("tmp", [128, 2, 512], F32, nslots=2)
    hm = P.sbuf("hm", [128, 2, D], F32, nslots=2)
    hmb = P.sbuf("hmb", [128, 2, D], BF16, nslots=2)
    hmT = P.sbuf("hmT", [128, 2, D], BF16, nslots=2)
    pG = P.psum("pG", [128, 2, 512], F32, nslots=2)
    pP = P.psum("pP", [128, 2, 512], F32, nslots=2)
    pTb = P.psum("pTb", [128, 2, 8, 128], BF16, nslots=2)
    cnt = 0

    def prefetch(t):
        for n in ("YA", "YB", "YC"):
            P.dma("sp", yT[n][t % 2], V(getattr(k, n).ap[t].rearrange("p a b -> p (a b)"), k.Y_b[n][t]))

    def finish(t):
        j = t % 2
        P.copy("pool", hmb[j], hm[j])
        for c in range(8):
            P.tr(pTb[j][:, c, :], hmb[j][:, c * 128:(c + 1) * 128], k.identb)
        P.copy("act", hmT[j], pTb[j].re("p a b -> p (a b)"))
        P.dma("sp", V(k.HM.ap[t], k.HM_b[t]), hmT[j], sb=hmT[j])

    prefetch(0)
    for t in range(NT):
        j = t % 2
        h_ = hT(k, t)
        if t + 1 < NT:
            prefetch(t + 1)
        for xi, n in enumerate(("YA", "YB", "YC")):
            for nb in range(2):
                if t > 0 and xi == 1 and nb == 0:
                    finish(t - 1)
                pg, pp = pG[cnt % 2], pP[cnt % 2]
                s_, t_ = sg[cnt % 2], tmp[cnt % 2]
                cnt += 1
                c0 = xi * 1024 + nb * 512
                for dc in range(8):
                    P.mm(pg, h_[:, dc, :], WG[:, dc, c0:c0 + 512], start=(dc == 0), stop=(dc == 7))
                for c in range(4):
                    P.mm(pp, yT[n][j][:, c * 128:(c + 1) * 128], WBR[:, xi, c, nb * 512:(nb + 1) * 512],
                         start=(c == 0), stop=(c == 3))
                P.act(s_, pg, AF.Sigmoid)
                if xi == 0:
                    P.tt("dve", hm[j][:, nb * 512:(nb + 1) * 512], pp, s_, ALU.mult)
                else:
                    P.tt("dve", t_, pp, s_, ALU.mult)
                    P.tt("pool", hm[j][:, nb * 512:(nb + 1) * 512], hm[j][:, nb * 512:(nb + 1) * 512], t_, ALU.add)
    finish(NT - 1)
    P.end()


def phaseM2(k, l):
    P = k.P
    P.begin("M2_%d" % l)
    WO = P.sbuf("WO", [128, 8, 1024], BF16)
    load_w(P, WO, k.w["w_out"][l], engs=("pool", "act", "dve"), nst=6)
    WR = P.sbuf("WR", [128, 8, 36], F32)
    for c in range(8):
        P.dma("sp", WR[:, c, :], k.w["wr_cat"][l][c * 128:(c + 1) * 128, :], sb=WR)
    rbias = P.sbuf("rbias", [128, 36], F32)
    P.dma("sp", rbias, brow(k.w["br_cat"][l], 36))
    g1 = P.sbuf("g1", [128, D], F32)
    b1 = P.sbuf("b1", [128, D], F32)
    P.dma("sp", g1, brow(k.w["ln1_g"][l], D))
    P.dma("sp", b1, brow(k.w["ln1_b"][l], D))
    hmT = P.sbuf("hmT", [128, 3, D], BF16, nslots=3)
    hres_t = P.sbuf("hres_t", [128, 3, D], F32, nslots=3)
    r = P.sbuf("r", [128, 2, D], F32, nslots=2)
    h1 = P.sbuf("h1", [128, 2, D], F32, nslots=2)
    hTf = P.sbuf("hTf", [128, 2, D], F32, nslots=2)
    rt_ = P.sbuf("rt_", [128, 2, 128], F32, nslots=2)
    ln = LN(k, "ln1", D, k.eps5)
    pX = P.psum("pX", [128, 2, 512], F32, nslots=2)
    pT = P.psum("pT", [128, 2, 8, 128], F32, nslots=2)
    pRt = P.psum("pRt", [128, 512], F32)

    def prefetch(t):
        P.dma("sp", hmT[t % 3], V(k.HM.ap[t], k.HM_b[t]))
        P.dma("sp", hres_t[t % 3], hres_tile(k, t))

    def stage1(t):
        j = t % 2
        for nb in range(2):
            for dc in range(8):
                P.mm(pX[nb], hmT[t % 3][:, dc * 128:(dc + 1) * 128], WO[:, dc, nb * 512:(nb + 1) * 512],
                     start=(dc == 0), stop=(dc == 7))
            P.stt("dve", r[j][:, nb * 512:(nb + 1) * 512], hres_t[t % 3][:, nb * 512:(nb + 1) * 512], float(DN_ALPHA),
                  pX[nb], ALU.mult, ALU.add)
        ln.part1(r[j], h1[j])

    def stage2(t):
        j = t % 2
        ln.part2(h1[j], g1, b1, "pool", "dve")
        P.dma("sp", hres_tile(k, t), h1[j], sb=h1[j])
        for c in range(8):
            P.tr(pT[j][:, c, :], h1[j][:, c * 128:(c + 1) * 128], k.ident)
        for hh in range(2):
            P.copy("act", hTf[j][:, hh * 512:(hh + 1) * 512], pT[j][:, hh * 4:(hh + 1) * 4, :].re("p a b -> p (a b)"))
        for hh in range(2):
            P.copy("act", hT(k, t)[:, hh * 4:(hh + 1) * 4, :], pT[j][:, hh * 4:(hh + 1) * 4, :])
        if not getattr(k, "no_router", False):
            router(k, t, hTf[j], WR, rbias, pRt, rt_[j])

    prefetch(0)
    prefetch(1)
    stage1(0)
    for t in range(NT):
        if t + 1 < NT:
            if t + 2 < NT:
                prefetch(t + 2)
            stage1(t + 1)
        stage2(t)
    P.end()


def router(k, t, hTf, WR, rbias, ps, s):
    P = k.P
    for dc in range(8):
        P.mm(ps[:, 0:36], hTf[:, dc * 128:(dc + 1) * 128], WR[:, dc, :], start=(dc == 0), stop=(dc == 7))
    lg = s[:, 0:36]
    P.tt("dve", lg, ps[:, 0:36], rbias, ALU.add)
    gl, el = s[:, 0:4], s[:, 4:36]
    gmax, gsum, gp, d21, e21, den, w1, w2 = (s[:, 40 + i:41 + i] for i in range(8))
    ge, oh, pen = s[:, 48:52], s[:, 52:56], s[:, 56:60]
    top8 = s[:, 60:68]
    elm = s[:, 68:100]
    P.reduce("dve", gmax, gl, ALU.max)
    P.ts("dve", ge, gl, gmax, None, op0=ALU.subtract)
    P.memset("dve", gsum, 0.0)
    P.act(ge, ge, AF.Exp, accum=gsum)
    P.generic("dve", lambda e: e.reciprocal(gp.ap, gsum.ap), [gsum], [gp])
    P.ts("dve", pen, gl, gmax, float(NEG), op0=ALU.is_lt, op1=ALU.mult)
    P.tt("dve", elm.re("p (g e) -> p g e", g=4), el.re("p (g e) -> p g e", g=4), pen.un(2).bc([128, 4, 8]), ALU.add)
    P.generic("dve", lambda e: e.max(top8.ap, elm.ap), [elm], [top8])
    m1, m2 = top8[:, 0:1], top8[:, 1:2]
    P.tt("dve", d21, m2, m1, ALU.subtract)
    P.act(e21, d21, AF.Exp)
    P.ts("dve", den, e21, 1.0, None, op0=ALU.add)
    P.generic("dve", lambda e: e.reciprocal(den.ap, den.ap), [den], [den])
    P.tt("dve", w1, den, gp, ALU.mult)
    P.tt("dve", w2, w1, e21, ALU.mult)
    cm = k.COMB[:, t, :]
    P.ts("dve", cm, elm, m1, w1, op0=ALU.is_equal, op1=ALU.mult)
    P.ts("dve", elm, elm, m2, w2, op0=ALU.is_equal, op1=ALU.mult)
    P.tt("dve", cm, cm, elm, ALU.add)


def phaseE(k, l, last):
    P = k.P
    P.begin("E%d" % l)
    HALF = NT // 2
    g2 = P.sbuf("g2", [128, D], F32)
    b2 = P.sbuf("b2", [128, D], F32)
    P.dma("sp", g2, brow(k.w["ln2_g"][l], D))
    P.dma("sp", b2, brow(k.w["ln2_b"][l], D))
    acc = P.sbuf("acc", [128, HALF, D], F32, nslots=HALF)
    import os
    NWS = 3 if USE_SWDGE else 2
    wg = P.sbuf("wg", [128, NWS, 8, 256], BF16, nslots=NWS)
    wu = P.sbuf("wu", [128, NWS, 8, 256], BF16, nslots=NWS)
    wd = P.sbuf("wd", [128, NWS, 2, 1024], BF16, nslots=NWS)
    sgt = P.sbuf("sgt", [128, 2, 512], BF16, nslots=2)
    hid = P.sbuf("hid", [128, 4, 512], BF16, nslots=4)
    h1t = P.sbuf("h1t", [128, 3, D], F32, nslots=3)
    ln = LN(k, "ln2", D, k.eps5)
    pGt = P.psum("pGt", [128, 2, 512], F32, nslots=2)
    pUp = P.psum("pUp", [128, 2, 512], F32, nslots=2)
    pOd = P.psum("pOd", [128, 4, 512], F32, nslots=4)
    pT = None
    w_gate, w_up, w_down = k.w["w_gate"][l], k.w["w_up"][l], k.w["w_down"][l]
    cg = [0]
    co = [0]

    def gate_up(s, tg):
        hids = []
        for fb in range(2):
            pg, pu = pGt[cg[0] % 2], pUp[cg[0] % 2]
            s_ = sgt[cg[0] % 2]
            hd = hid[cg[0] % 4]
            cg[0] += 1
            for dc in range(8):
                P.mm(pg, wg[s][:, dc, fb * 128:(fb + 1) * 128], hT_span(k, dc, tg, 4), start=(dc == 0), stop=(dc == 7))
            for dc in range(8):
                P.mm(pu, wu[s][:, dc, fb * 128:(fb + 1) * 128], hT_span(k, dc, tg, 4), start=(dc == 0), stop=(dc == 7))
            P.act(s_, pg, AF.Silu)
            P.tt("dve", hd, pu, s_, ALU.mult)
            hids.append(hd)
        return hids

    def down(s, e, tg, t0, hids):
        for ti in range(4):
            t = tg + ti
            pos = []
            for nb in range(2):
                pos.append(pOd[co[0] % 4])
                co[0] += 1
            if ti == 0:
                order = [(0, 0), (1, 0), (0, 1), (1, 1)]
            else:
                order = [(0, 0), (0, 1), (1, 0), (1, 1)]
            for nb, fb in order:
                P.mm(pos[nb], hids[fb][:, ti * 128:(ti + 1) * 128], wd[s][:, fb, nb * 512:(nb + 1) * 512],
                     start=(fb == 0), stop=(fb == 1))
            for nb in range(2):
                a_ = acc[t - t0][:, nb * 512:(nb + 1) * 512]
                cw = k.COMB[:, t, e:e + 1]
                if e == 0:
                    P.ts("dve", a_, pos[nb], cw, None, op0=ALU.mult)
                else:
                    P.stt("dve", a_, pos[nb], cw, a_, ALU.mult, ALU.add)

    for half in range(2):
        t0 = half * HALF
        for e in range(32):
            s = (half * 32 + e) % NWS
            load_w(P, wg[s], w_gate[e], engs=("pool", "act"))
            load_w(P, wu[s], w_up[e], engs=("pool", "act"))
            load_w(P, wd[s], w_down[e], engs=("pool", "act"))
            for grp in range(HALF // 4):
                tg = t0 + grp * 4
                hids = gate_up(s, tg)
                down(s, e, tg, t0, hids)
        def tail1(ti):
            t = t0 + ti
            a_ = acc[ti]
            P.stt("dve", a_, h1t[ti % 3], float(DN_ALPHA), a_, ALU.mult, ALU.add)
            ln.part1(a_, a_)

        def tail1b(ti):
            t = t0 + ti
            a_ = acc[ti]
            ln.part2(a_, g2, b2, "pool", "dve")
            if last:
                P.dma("sp", V(k.out.ap[t * 128:(t + 1) * 128, :], Buf("o%d" % t)), a_, sb=a_)
            else:
                P.dma("sp", hres_tile(k, t), a_, sb=a_)

        def tail2(ti):
            t = t0 + ti
            a_ = acc[ti]
            pt = (pOd[0], pOd[1]) if ti % 2 == 0 else (pOd[2], pOd[3])
            for c in range(8):
                P.tr(pt[c // 4][:, (c % 4) * 128:(c % 4 + 1) * 128], a_[:, c * 128:(c + 1) * 128], k.ident)
            for hh in range(2):
                P.copy("act", hT(k, t)[:, hh * 4:(hh + 1) * 4, :], pt[hh].re("p (a b) -> p a b", a=4))

        P.dma("sp", h1t[0], hres_tile(k, t0))
        P.dma("sp", h1t[1], hres_tile(k, t0 + 1))
        tail1(0)
        for ti in range(HALF):
            if ti + 2 < HALF:
                P.dma("sp", h1t[(ti + 2) % 3], hres_tile(k, t0 + ti + 2))
            if ti + 1 < HALF:
                tail1(ti + 1)
            tail1b(ti)
            if not last:
                tail2(ti)
    P.end()


_CACHE = {}


def make_in_maps(inputs, n_cores=8):
    consts = host_consts()
    inputs = dict(inputs)
    inputs["wr_cat"] = np.concatenate([np.asarray(inputs["w_rg"]), np.asarray(inputs["w_re"])], axis=-1)
    inputs["br_cat"] = np.concatenate([np.asarray(inputs["b_rg"]), np.asarray(inputs["b_re"])], axis=-1)
    shared = {n: np.ascontiguousarray(np.asarray(inputs[n], dtype=np.float32)) for n, _ in WNAMES}
    shared.update(consts)
    maps = []
    x = np.asarray(inputs["x"], dtype=np.float32)
    pos = np.asarray(inputs["positions"]).astype(np.int32)
    for b in range(n_cores):
        m = dict(shared)
        m["x"] = np.ascontiguousarray(x[b])
        m["pos"] = np.ascontiguousarray(pos[b].reshape(NT, 128).T)
        maps.append(m)
    return maps


def kernel(**inputs):
    if "nc" not in _CACHE:
        _CACHE["nc"] = build()[0]
    nc = _CACHE["nc"]
    maps = make_in_maps(inputs, 8)
    res = run_bass_kernel_spmd(nc, maps, core_ids=list(range(8)))
    return np.stack([np.asarray(r["out"], dtype=np.float32) for r in res.results], axis=0)
```

```python
import numpy as np
from contextlib import ExitStack
import concourse.bass as bass
import concourse.mybir as mybir
from concourse.bass_utils import run_bass_kernel_spmd

F32 = mybir.dt.float32
BF16 = mybir.dt.bfloat16
I32 = mybir.dt.int32
AF = mybir.ActivationFunctionType
ALU = mybir.AluOpType
AX = mybir.AxisListType


class Buf:
    __slots__ = ("name", "last_write", "readers", "dstream")

    def __init__(self, name):
        self.name = name
        self.last_write = None
        self.readers = []
        self.dstream = None


class V:
    __slots__ = ("ap", "buf")

    def __init__(self, ap, buf):
        self.ap = ap
        self.buf = buf

    def __getitem__(self, idx):
        return V(self.ap[idx], self.buf)

    def re(self, pat, **kw):
        return V(self.ap.rearrange(pat, **kw), self.buf)

    def bc(self, shape):
        return V(self.ap.to_broadcast(list(shape)), self.buf)

    def un(self, axis):
        return V(self.ap.unsqueeze(axis), self.buf)

    def wb(self, bufs):
        return V(self.ap, bufs)


class Op:
    __slots__ = ("eng", "fn", "deps", "is_dma", "stream", "signal", "count", "tag")


class Stream:
    def __init__(self, name, inc):
        self.name = name
        self.inc = inc
        self.total = 0
        self.sem = None


NDSEM = 90
STRICT = True
import os
USE_SWDGE = bool(os.environ.get("USE_SWDGE"))
ENGS = ("pe", "act", "dve", "pool", "sp")
ENGOBJ = {"pe": "tensor", "act": "scalar", "dve": "vector", "pool": "gpsimd", "sp": "sync"}


def _bufs(vs):
    out = []
    for v in vs:
        b = v.buf if isinstance(v, V) else v
        if isinstance(b, (tuple, list)):
            for x in b:
                if x not in out:
                    out.append(x)
        elif b not in out:
            out.append(b)
    return out


class Prog:
    def __init__(self, nc):
        self.nc = nc
        self.ges = ExitStack()
        self.estream = {e: Stream("s_" + e, 1) for e in ENGS}
        for e in ENGS:
            self.estream[e].sem = self.ges.enter_context(nc.semaphore("s_" + e))
        self.dsem_pool = [self.ges.enter_context(nc.semaphore("dsem%d" % i)) for i in range(NDSEM)]
        self.dsem_next = 0
        self.dsem_base = [0] * NDSEM
        self.nphase = 0
        self.tot_ops = 0
        self.tot_waits = 0
        self.pes = None

    def _es(self, glob):
        return self.ges if glob else self.pes

    def sbuf(self, name, shape, dtype, nslots=None, glob=False):
        if not glob:
            name = self.pname + "_" + name
        t = self._es(glob).enter_context(self.nc.sbuf_tensor(name, list(shape), dtype))
        if nslots is None:
            return V(t[:], Buf(name))
        return [V(t[:, i], Buf(f"{name}{i}")) for i in range(nslots)]

    def psum(self, name, shape, dtype=F32, nslots=None):
        name = self.pname + "_" + name
        t = self.pes.enter_context(self.nc.psum_tensor(name, list(shape), dtype))
        if nslots is None:
            return V(t[:], Buf(name))
        return [V(t[:, i], Buf(f"{name}{i}")) for i in range(nslots)]

    def dram(self, name, shape, dtype, kind="Internal"):
        t = self.nc.dram_tensor(name, list(shape), dtype, kind=kind)
        return V(t.ap(), Buf(name))

    def begin(self, name):
        self.pname = name
        self.pes = ExitStack()
        self.ops = {e: [] for e in ENGS}
        self.all_ops = []
        self.dstreams = []
        self.touched = []

    def _rec(self, eng, fn, reads, writes, is_dma=False, dbuf=None, tag=""):
        op = Op()
        op.eng = eng
        op.fn = fn
        op.is_dma = is_dma
        op.signal = is_dma
        op.count = 0
        op.tag = tag
        if is_dma:
            b = _bufs([dbuf])[0]
            if b.dstream is None:
                b.dstream = Stream("d%d_%s" % (self.nphase, b.name), 16)
                self.dstreams.append(b.dstream)
                self.touched.append(b)
            op.stream = b.dstream
        else:
            op.stream = self.estream[eng]
        rb = _bufs(reads)
        wb = _bufs(writes)
        deps = []
        for b in rb:
            lw = b.last_write
            if lw is not None:
                deps.append((lw, "raw"))
        for b in wb:
            lw = b.last_write
            if lw is not None:
                deps.append((lw, "waw"))
            for r in b.readers:
                deps.append((r, "war"))
        fdeps = []
        for d, kind in deps:
            if d is op:
                continue
            if (not d.is_dma) and (not is_dma) and d.eng == eng:
                if eng == "pe" or (kind != "raw" and not STRICT):
                    continue
            if d.is_dma and is_dma and d.stream is op.stream and kind == "waw":
                continue
            fdeps.append(d)
        op.deps = fdeps
        for b in rb:
            b.readers.append(op)
            self.touched.append(b)
        for b in wb:
            b.last_write = op
            b.readers = []
            self.touched.append(b)
        self.ops[eng].append(op)
        self.all_ops.append(op)
        return op

    def mm(self, out, lhsT, rhs, start=True, stop=True, **kw):
        return self._rec("pe", lambda e: e.matmul(out.ap, lhsT.ap, rhs.ap, start=start, stop=stop, **kw),
                         [lhsT, rhs] + ([] if start else [out]), [out])

    def tr(self, out, in_, ident):
        return self._rec("pe", lambda e: e.transpose(out.ap, in_.ap, ident.ap), [in_, ident], [out])

    def act(self, out, in_, func, bias=None, scale=None, accum=None):
        reads = [in_]
        kw = {}
        if bias is not None:
            if isinstance(bias, V):
                reads.append(bias)
                kw["bias"] = bias.ap
            else:
                kw["bias"] = bias
        if scale is not None:
            if isinstance(scale, V):
                reads.append(scale)
                kw["scale"] = scale.ap
            else:
                kw["scale"] = scale
        writes = [out]
        if accum is not None:
            kw["accum_out"] = accum.ap
            writes.append(accum)
        return self._rec("act", lambda e: e.activation(out.ap, in_.ap, func, **kw), reads, writes)

    def tt(self, eng, out, a, b, op):
        return self._rec(eng, lambda e: e.tensor_tensor(out.ap, a.ap, b.ap, op), [a, b], [out])

    def ts(self, eng, out, a, s1, s2=None, op0=ALU.mult, op1=None, accum=None):
        reads = [a]
        s1a = s1.ap if isinstance(s1, V) else s1
        s2a = s2.ap if isinstance(s2, V) else s2
        if isinstance(s1, V):
            reads.append(s1)
        if isinstance(s2, V):
            reads.append(s2)
        writes = [out]
        kw = {}
        if op1 is not None:
            kw["op1"] = op1
        if accum is not None:
            kw["accum_out"] = accum.ap
            writes.append(accum)
        return self._rec(eng, lambda e: e.tensor_scalar(out.ap, a.ap, s1a, s2a, op0, **kw), reads, writes)

    def stt(self, eng, out, a, s, b, op0, op1):
        reads = [a, b]
        sa = s.ap if isinstance(s, V) else s
        if isinstance(s, V):
            reads.append(s)
        return self._rec(eng, lambda e: e.scalar_tensor_tensor(out.ap, a.ap, sa, b.ap, op0, op1), reads, [out])

    def copy(self, eng, out, in_):
        if eng == "act":
            return self._rec("act", lambda e: e.copy(out.ap, in_.ap), [in_], [out])
        return self._rec(eng, lambda e: e.tensor_copy(out.ap, in_.ap), [in_], [out])

    def memset(self, eng, out, val):
        return self._rec(eng, lambda e: e.memset(out.ap, val), [], [out])

    def reduce(self, eng, out, in_, op, axis=AX.X):
        return self._rec(eng, lambda e: e.tensor_reduce(out.ap, in_.ap, axis, op), [in_], [out])

    def generic(self, eng, fn, reads, writes):
        return self._rec(eng, fn, reads, writes)

    def dma(self, q, out, in_, sb=None, **kw):
        if sb is None:
            sb = out
        return self._rec(q, lambda e: e.dma_start(out.ap, in_.ap, **kw), [in_], [out], is_dma=True, dbuf=sb)

    def end(self):
        nc = self.nc
        for op in self.all_ops:
            for d in op.deps:
                d.signal = True
        for e in ENGS:
            for op in reversed(self.ops[e]):
                if not op.is_dma:
                    op.signal = True
                    break
        start_tot = {e: self.estream[e].total for e in ENGS}
        assert len(self.dstreams) <= NDSEM, len(self.dstreams)
        for s in self.dstreams:
            s.idx = self.dsem_next % NDSEM
            self.dsem_next += 1
            s.sem = self.dsem_pool[s.idx]
            s.total = self.dsem_base[s.idx]
            s.base = s.total
        for e in ENGS:
            for op in self.ops[e]:
                if op.signal:
                    st = op.stream
                    st.total += st.inc
                    op.count = st.total
        end_tot = {e: self.estream[e].total for e in ENGS}
        nwaits = 0
        with nc.Block() as block:
            def make(ename):
                def body(e):
                    nonlocal nwaits
                    seen = {}
                    for x in ENGS:
                        if x != ename and start_tot[x] > 0:
                            e.wait_ge(self.estream[x].sem, start_tot[x])
                        seen[self.estream[x]] = start_tot[x]
                    for op in self.ops[ename]:
                        need = {}
                        for d in op.deps:
                            st = d.stream
                            if d.count > seen.get(st, 0) and d.count > need.get(st, 0):
                                need[st] = d.count
                        for st, c in need.items():
                            e.wait_ge(st.sem, c)
                            seen[st] = c
                            nwaits += 1
                        ins = op.fn(e)
                        if op.signal:
                            ins.then_inc(op.stream.sem, op.stream.inc)
                    if ename == "sp":
                        for s in self.dstreams:
                            if s.total > s.base:
                                e.wait_ge(s.sem, s.total)
                        for x in ENGS:
                            if x != "sp" and end_tot[x] > start_tot[x]:
                                e.wait_ge(self.estream[x].sem, end_tot[x])
                        st = self.estream["sp"]
                        st.total += 1
                        e.sem_inc(st.sem, 1)
                return body
            for ename in ENGS:
                getattr(block, ENGOBJ[ename])(make(ename))
        for s in self.dstreams:
            self.dsem_base[s.idx] = s.total
        for b in self.touched:
            b.last_write = None
            b.readers = []
            b.dstream = None
        self.tot_ops += len(self.all_ops)
        self.tot_waits += nwaits
        self.nphase += 1
        print('phase', self.pname, 'ops', len(self.all_ops), 'waits', nwaits, 'totals', {e: self.estream[e].total for e in ENGS}, 'ndma_streams', len(self.dstreams), 'sbuf_free', self.nc.sbuf_bytes_remaining, flush=True)
        self.pes.close()
        self.pes = None

    def finish(self):
        self.ges.close()

D = 1024
SEQ = 4096
NT = SEQ // 128
DEPTH = 2
N_IN = 6612
DN_ALPHA = (2 * DEPTH) ** 0.25
NIT = 18
NEG = -1.0e30
TWO_PI = 6.283185307179586
C1 = 6.28125
C2 = TWO_PI - C1

WNAMES = [("ln_in_g", [D]), ("ln_in_b", [D]), ("w_in", [DEPTH, D, N_IN]), ("idx_k_g", [DEPTH, 64]),
          ("gla_wa2", [DEPTH, 16, 256]), ("gla_ba", [DEPTH, 256]), ("gla_norm_g", [DEPTH, 128]),
          ("gm_ln_g", [DEPTH, 512]), ("gm_ln_b", [DEPTH, 512]), ("gm_ws", [DEPTH, 4, 128, 128]),
          ("gm_bs", [DEPTH, 4, 128]), ("w_branch_a", [DEPTH, 512, D]), ("w_branch_b", [DEPTH, 512, D]),
          ("w_branch_c", [DEPTH, 512, D]), ("w_out", [DEPTH, D, D]), ("ln1_g", [DEPTH, D]), ("ln1_b", [DEPTH, D]),
          ("wr_cat", [DEPTH, D, 36]), ("br_cat", [DEPTH, 36]),
          ("w_gate", [DEPTH, 32, D, 256]), ("w_up", [DEPTH, 32, D, 256]), ("w_down", [DEPTH, 32, 256, D]),
          ("ln2_g", [DEPTH, D]), ("ln2_b", [DEPTH, D])]


def host_consts():
    ident = np.eye(128, dtype=np.float32)
    triu = np.triu(np.ones((128, 128), np.float32))
    cbias = np.where(np.arange(128)[None, :] <= np.arange(128)[:, None], 0.0, NEG).astype(np.float32)
    invf = (500000.0 ** (-np.arange(0, 16, 2, dtype=np.float32) / 16)).astype(np.float32)
    invf = np.broadcast_to(invf[None, :], (128, 8)).copy()
    pow2 = np.broadcast_to((0.5 ** np.arange(1, NIT + 1))[None, :], (128, NIT)).astype(np.float32).copy()
    return {"c_ident": ident, "c_triu": triu, "c_cbias": cbias, "c_invf": invf, "c_pow2": pow2}


def brow(v, n):
    return v.re("(o n) -> o n", o=1).bc([128, n])


class K:
    pass


def build(debug=False, stop_after=None, layers=DEPTH):
    nc = bass.Bass("TRN2", target_bir_lowering=False)
    P = Prog(nc)
    k = K()
    k.P = P
    k.debug = debug
    k.skip_abc = stop_after if stop_after in ("M1only", "M2only", "CM1", "BCM1", "AM1") else None
    import os
    k.no_router = bool(os.environ.get("NO_ROUTER"))
    k.stop_m1 = (stop_after == "M1")
    if stop_after == "M1":
        stop_after = "M"
    dk = "ExternalOutput" if debug else "Internal"
    k.x = P.dram("x", [SEQ, D], F32, kind="ExternalInput")
    k.pos = P.dram("pos", [128, NT], I32, kind="ExternalInput")
    k.w = {}
    for n, shp in WNAMES:
        k.w[n] = P.dram(n, shp, F32, kind="ExternalInput")
    k.c = {}
    for n, a in host_consts().items():
        k.c[n] = P.dram(n, list(a.shape), F32, kind="ExternalInput")
    k.out = P.dram("out", [SEQ, D], F32, kind="ExternalOutput")
    k.hres = P.dram("hres", [SEQ, D], F32, kind=dk)
    k.YA = P.dram("YA", [NT, 128, 4, 128], BF16, kind=dk)
    k.YB = P.dram("YB", [NT, 128, 4, 128], BF16, kind=dk)
    k.YC = P.dram("YC", [NT, 128, 4, 128], BF16, kind=dk)
    k.hres_b = [Buf("hres%d" % t) for t in range(NT)]
    k.HM = P.dram("HM", [NT, 128, D], BF16, kind=dk)
    k.HM_b = [Buf("HM%d" % t) for t in range(NT)]
    k.Y_b = {n: [Buf("%s%d" % (n, t)) for t in range(NT)] for n in ("YA", "YB", "YC")}

    hT_all = P.sbuf("hT", [128, 8, SEQ], BF16, glob=True)
    k.hT_b = [Buf("hT%d" % t) for t in range(NT)]
    k.hT_all = hT_all
    k.ident = P.sbuf("ident", [128, 128], F32, glob=True)
    k.identb = P.sbuf("identb", [128, 128], BF16, glob=True)
    k.triu = P.sbuf("triu", [128, 128], F32, glob=True)
    k.cbias = P.sbuf("cbias", [128, 128], F32, glob=True)
    k.ones = P.sbuf("ones", [128, 128], F32, glob=True)
    k.onesb = P.sbuf("onesb", [128, 128], BF16, glob=True)
    k.eps5 = P.sbuf("eps5", [128, 1], F32, glob=True)
    k.eps6 = P.sbuf("eps6", [128, 1], F32, glob=True)
    k.COS = P.sbuf("COS", [128, NT, 8], F32, glob=True)
    k.SIN = P.sbuf("SIN", [128, NT, 8], F32, glob=True)
    k.COMB = P.sbuf("COMB", [128, NT, 32], F32, glob=True)
    k.pow2 = P.sbuf("pow2", [128, NIT], F32, glob=True)

    phase0(k)
    if stop_after == "p0":
        return fin(k)
    for l in range(layers):
        if k.skip_abc:
            if k.skip_abc == "M1only":
                phaseM1(k, l)
            if k.skip_abc == "M2only":
                phaseM2(k, l)
            if k.skip_abc == "CM1":
                phaseC(k, l); phaseM1(k, l)
            if k.skip_abc == "BCM1":
                phaseB(k, l); phaseC(k, l); phaseM1(k, l)
            if k.skip_abc == "AM1":
                phaseA(k, l); phaseM1(k, l)
            return fin(k)
        phaseA(k, l)
        if stop_after == "A":
            return fin(k)
        phaseB(k, l)
        if stop_after == "B":
            return fin(k)
        phaseC(k, l)
        if stop_after == "C":
            return fin(k)
        phaseM(k, l)
        if stop_after == "M":
            return fin(k)
        phaseE(k, l, last=(l == layers - 1))
    return fin(k)


def fin(k):
    k.P.finish()
    return k.P.nc, k.P


def hT(k, t):
    return V(k.hT_all.ap[:, :, t * 128:(t + 1) * 128], k.hT_b[t])


def hT_span(k, dc, t0, nt):
    return V(k.hT_all.ap[:, dc, t0 * 128:(t0 + nt) * 128], tuple(k.hT_b[t0:t0 + nt]))


def hres_tile(k, t):
    return V(k.hres.ap[t * 128:(t + 1) * 128, :], k.hres_b[t])


def load_w(P, dst, src, q="pool", engs=("pool",), nst=3):
    n = src.ap.shape[1]
    import os
    if not USE_SWDGE:
        if not hasattr(P, "_stage") or P._stage_phase != P.nphase:
            P._stage = P.sbuf("wstage", [128, nst, 1024], F32, nslots=nst)
            P._stage_phase = P.nphase
            P._stage_i = 0
        nch = src.ap.shape[0] // 128
        ns = len(P._stage)
        if n < 1024 and 1024 % n == 0 and nch % (1024 // n) == 0 and len(dst.ap.shape) == 3:
            G = 1024 // n
            for c in range(0, nch, G):
                st = P._stage[P._stage_i % ns]
                ce = engs[P._stage_i % len(engs)]
                P._stage_i += 1
                P.dma("sp", st.re("p (g n) -> p g n", g=G), src[c * 128:(c + G) * 128, :].re("(g p) n -> p g n", p=128), sb=st)
                P.copy(ce, dst[:, c:c + G, :], st.re("p (g n) -> p g n", g=G))
            return
        for c0 in range(0, n, 1024):
            for c in range(nch):
                c1 = min(n, c0 + 1024)
                st = P._stage[P._stage_i % ns]
                ce = engs[P._stage_i % len(engs)]
                P._stage_i += 1
                P.dma("sp", st[:, 0:c1 - c0], src[c * 128:(c + 1) * 128, c0:c1], sb=st)
                P.copy(ce, dst[:, c, c0:c1], st[:, 0:c1 - c0])
        return
    for c0 in range(0, n, 1024):
        c1 = min(n, c0 + 1024)
        P.dma(q, dst[:, :, c0:c1], src[:, c0:c1].re("(c p) n -> p c n", p=128), sb=dst)


class LN:
    def __init__(self, k, name, Dn, eps_t, nb=2):
        P = k.P
        self.k = k
        self.Dn = Dn
        self.nch = max(1, Dn // 512)
        self.cw = min(Dn, 512)
        self.st = P.sbuf(name + "_st", [128, nb, self.nch * 6], F32, nslots=nb)
        self.mv = P.sbuf(name + "_mv", [128, nb, 2], F32, nslots=nb)
        self.lv = P.sbuf(name + "_lv", [128, nb, 1], F32, nslots=nb)
        self.rs = P.sbuf(name + "_rs", [128, nb, 1], F32, nslots=nb)
        self.eps = eps_t
        self.nb = nb
        self.i = 0

    def __call__(self, r, y, gbc=None, bbc=None, eng_aff="pool", eng_bias=None):
        self.part1(r, y)
        self.part2(y, gbc, bbc, eng_aff, eng_bias)

    def part2(self, y, gbc=None, bbc=None, eng_aff="pool", eng_bias=None):
        P = self.k.P
        if gbc is not None:
            P.tt(eng_aff, y, y, gbc, ALU.mult)
        if bbc is not None:
            P.tt(eng_bias or eng_aff, y, y, bbc, ALU.add)

    def part1(self, r, y):
        P = self.k.P
        j = self.i % self.nb
        self.i += 1
        st, mv, lv, rs = self.st[j], self.mv[j], self.lv[j], self.rs[j]
        for c in range(self.nch):
            P.generic("dve", lambda e, c=c: e.bn_stats(st.ap[:, c * 6:(c + 1) * 6], r.ap[:, c * self.cw:(c + 1) * self.cw]),
                      [r], [st])
        P.generic("dve", lambda e: e.bn_aggr(mv.ap, st.ap), [st], [mv])
        P.act(lv, mv[:, 1:2], AF.Ln, bias=self.eps, scale=1.0)
        P.act(rs, lv, AF.Exp, scale=-0.5)
        P.ts("dve", y, r, mv[:, 0:1], rs, op0=ALU.subtract, op1=ALU.mult)


def phase0(k):
    P = k.P
    P.begin("p0")
    for name, t in (("c_ident", k.ident), ("c_triu", k.triu), ("c_cbias", k.cbias), ("c_pow2", k.pow2)):
        P.dma("sp", t, k.c[name])
    P.copy("dve", k.identb, k.ident)
    P.memset("pool", k.ones, 1.0)
    P.memset("pool", k.onesb, 1.0)
    P.memset("pool", k.eps5, 1e-5)
    P.memset("pool", k.eps6, 1e-6)
    posi = P.sbuf("posi", [128, NT], I32)
    posf = P.sbuf("posf", [128, NT], F32)
    invf = P.sbuf("invf", [128, 8], F32)
    ang = P.sbuf("ang", [128, NT, 8], F32)
    a2 = P.sbuf("a2", [128, NT, 8], F32)
    kf = P.sbuf("kf", [128, NT, 8], F32)
    ki = P.sbuf("ki", [128, NT, 8], I32)
    P.dma("sp", posi, k.pos)
    P.dma("sp", invf, k.c["c_invf"])
    P.copy("dve", posf, posi)
    P.tt("dve", ang, posf.un(2).bc([128, NT, 8]), invf.un(1).bc([128, NT, 8]), ALU.mult)
    for tab, shift in ((k.SIN, 0.0), (k.COS, np.pi / 2)):
        if shift != 0.0:
            P.ts("dve", a2, ang, float(shift), None, op0=ALU.add)
            src = a2
        else:
            src = ang
        P.ts("dve", kf, src, float(1.0 / TWO_PI), None, op0=ALU.mult)
        P.copy("dve", ki, kf)
        P.copy("dve", kf, ki)
        P.stt("dve", a2, kf, float(-C1), src, ALU.mult, ALU.add)
        P.stt("dve", a2, kf, float(-C2), a2, ALU.mult, ALU.add)
        P.ts("dve", a2, a2, float(np.pi), float(-np.pi), op0=ALU.min, op1=ALU.max)
        P.act(tab, a2, AF.Sin)
    gbc = P.sbuf("gin", [128, D], F32)
    bbc = P.sbuf("bin", [128, D], F32)
    P.dma("sp", gbc, brow(k.w["ln_in_g"], D))
    P.dma("sp", bbc, brow(k.w["ln_in_b"], D))
    xt = P.sbuf("xt", [128, 2, D], F32, nslots=2)
    yt = P.sbuf("yt", [128, 2, D], F32, nslots=2)
    pT = P.psum("pT", [128, 2, 8, 128], F32, nslots=2)
    ln = LN(k, "ln0", D, k.eps5)
    P.dma("sp", xt[0], k.x[0:128, :])
    P.dma("sp", xt[1], k.x[128:256, :])
    ln.part1(xt[0], yt[0])
    for t in range(NT):
        j = t % 2
        if t + 1 < NT:
            ln.part1(xt[(t + 1) % 2], yt[(t + 1) % 2])
        if t + 2 < NT:
            P.dma("sp", xt[j], k.x[(t + 2) * 128:(t + 3) * 128, :])
        ln.part2(yt[j], gbc, bbc, "pool", "dve")
        P.dma("sp", hres_tile(k, t), yt[j], sb=yt[j])
        for c in range(8):
            P.tr(pT[j][:, c, :], yt[j][:, c * 128:(c + 1) * 128], k.ident)
        P.copy("act", hT(k, t), pT[j])
    P.end()


def phaseA(k, l):
    P = k.P
    P.begin("A%d" % l)
    w_in = k.w["w_in"][l]
    WA = P.sbuf("WA", [128, 8, 964], BF16)
    load_w(P, WA, w_in[:, 0:964], engs=("pool", "act", "dve"))
    gik = P.sbuf("gik", [128, 64], F32)
    P.dma("sp", gik, brow(k.w["idx_k_g"][l], 64))
    ident4 = P.sbuf("ident4", [128, 4, 128], BF16)
    P.copy("pool", ident4, k.identb.un(1).bc([128, 4, 128]))
    id4 = ident4.re("p a b -> p (a b)")
    kT = P.sbuf("kT", [64, SEQ], BF16)
    kiT = P.sbuf("kiT", [64, SEQ], BF16)
    kT_b = [Buf("kT%d" % t) for t in range(NT)]
    kiT_b = [Buf("kiT%d" % t) for t in range(NT)]
    V1 = P.sbuf("V1", [128, NT, 65], BF16)
    V1_b = [Buf("V1_%d" % t) for t in range(NT)]
    P.memset("pool", V1.wb(tuple(V1_b)), 1.0)
    X = P.sbuf("X", [128, 2, 964], F32, nslots=2)
    Xr = P.sbuf("Xr", [128, 2, 960], BF16, nslots=2)
    rt = P.sbuf("rt", [128, 2, 4, 15, 8], F32, nslots=2)
    wsc = P.sbuf("wsc", [128, 2, 4], F32, nslots=2)
    lns = P.sbuf("lns", [128, 2, 8], F32, nslots=2)
    j64 = P.sbuf("j64", [128, 64], F32)
    qTb = P.sbuf("qTb", [64, 3, 1024], BF16, nslots=3)
    qiTb = P.sbuf("qiTb", [64, 2, 512], BF16, nslots=2)
    S = P.sbuf("S", [128, 2, SEQ], F32, nslots=2)
    rl = P.sbuf("rl", [128, 3, 512], F32, nslots=3)
    mb = P.sbuf("mb", [128, 2, SEQ], BF16, nslots=2)
    bs = P.sbuf("bs", [128, 2, 8], F32, nslots=2)
    cn = P.sbuf("cn", [128, 2, NIT], F32, nslots=2)
    halves = P.sbuf("halves", [128, 2, NIT], F32, nslots=2)
    E = P.sbuf("E", [128, 2, 1024], BF16, nslots=2)
    rec = P.sbuf("rec", [128, 2, 8], F32, nslots=2)
    ya = P.sbuf("ya", [128, 2, 512], BF16, nslots=2)
    yaT = P.sbuf("yaT", [128, 2, 512], BF16, nslots=2)
    pAS = P.psum("pAS", [128, 2, 512], F32, nslots=2)
    pT = P.psum("pT", [128, 16, 128], BF16)
    pL = P.psum("pL", [128, 2, 512], F32, nslots=2)
    pO = P.psum("pO", [128, 2, 512], F32, nslots=2)
    cnt = [0]

    def front_units(qb):
        j = qb % 2
        nk = (qb + 1) * 128
        Xj, Xrj = X[j], Xr[j]
        X3 = Xj[:, 0:960].re("p (s d) -> p s d", d=64)
        R3 = Xrj.re("p (s d) -> p s d", d=64)
        Sj = S[j]
        units = []

        def u_proj():
            for (ps, c0, c1) in ((pAS[0], 0, 512), (pAS[1], 512, 964)):
                for dc in range(8):
                    P.mm(ps[:, 0:c1 - c0], hT(k, qb)[:, dc, :], WA[:, dc, c0:c1], start=(dc == 0), stop=(dc == 7))
                P.copy("act", Xj[:, c0:c1], ps[:, 0:c1 - c0])
        units.append(u_proj)

        def u_ln_rope():
            xs = Xj[:, 896:960]
            ls = lns[j]
            s1, s2, m2, bb, lv, rs, nm, nmr = (ls[:, i:i + 1] for i in range(8))
            P.memset("pool", ls[:, 0:2], 0.0)
            P.act(j64, xs, AF.Identity, accum=s1)
            P.act(j64, xs, AF.Square, accum=s2)
            P.act(m2, s1, AF.Square, scale=1.0 / 64)
            P.act(bb, m2, AF.Identity, bias=k.eps5, scale=-1.0)
            P.act(lv, s2, AF.Ln, bias=bb, scale=1.0 / 64)
            P.act(rs, lv, AF.Exp, scale=-0.5)
            P.act(nm, s1, AF.Identity, scale=-1.0 / 64)
            P.act(nmr, nm, AF.Identity, scale=rs)
            P.act(xs, xs, AF.Identity, bias=nmr, scale=rs)
            P.tt("pool", xs, xs, gik, ALU.mult)
            P._rec("pool", lambda e: e.tensor_copy(V1.ap[:, qb, 0:64], Xj.ap[:, 576:640]), [Xj], [V1_b[qb]])
            P.ts("pool", wsc[j], Xj[:, 960:964], 0.0625, None, op0=ALU.mult)
            cb = k.COS[:, qb, :].un(1).bc([128, 15, 8])
            sb_ = k.SIN[:, qb, :].un(1).bc([128, 15, 8])
            t1, t2, t3, t4 = (rt[j][:, i] for i in range(4))
            P.copy("pool", R3[:, :, 16:64], X3[:, :, 16:64])
            P.tt("pool", t1, X3[:, :, 0:8], cb, ALU.mult)
            P.tt("pool", t2, X3[:, :, 8:16], sb_, ALU.mult)
            P.tt("pool", t3, X3[:, :, 0:8], sb_, ALU.mult)
            P.tt("pool", t4, X3[:, :, 8:16], cb, ALU.mult)
            P.tt("pool", R3[:, :, 0:8], t1, t2, ALU.subtract)
            P.tt("pool", R3[:, :, 8:16], t3, t4, ALU.add)
        units.append(u_ln_rope)

        def u_tr():
            slots = list(range(0, 9)) + list(range(10, 15))
            for i, s in enumerate(slots):
                P.tr(pT[0:64, i, :], R3[:, s, :], k.identb)
            P.copy("act", qTb[qb % 3], pT[0:64, 0:8, :].re("p a b -> p (a b)"))
            P.copy("act", V(kT.ap[:, qb * 128:(qb + 1) * 128], kT_b[qb]), pT[0:64, 8, :])
            P.copy("act", qiTb[j], pT[0:64, 9:13, :].re("p a b -> p (a b)"))
            P.copy("act", V(kiT.ap[:, qb * 128:(qb + 1) * 128], kiT_b[qb]), pT[0:64, 13, :])
        units.append(u_tr)

        nch = (nk + 511) // 512
        for ch in range(nch):
            for h in range(4):
                def u_s(ch=ch, h=h):
                    kw = min(512, nk - ch * 512)
                    tb = tuple(kiT_b[ch * 4:ch * 4 + kw // 128])
                    sl_ = Sj[:, ch * 512:ch * 512 + kw]
                    ps = pAS[cnt[0] % 2]
                    r_ = rl[cnt[0] % 3]
                    cnt[0] += 1
                    P.mm(ps[:, 0:kw], qiTb[j][:, h * 128:(h + 1) * 128], V(kiT.ap[:, ch * 512:ch * 512 + kw], tb))
                    P.act(r_[:, 0:kw], ps[:, 0:kw], AF.Relu)
                    wb_ = wsc[j][:, h:h + 1].bc([128, kw])
                    if h == 0:
                        P.tt("pool", sl_, r_[:, 0:kw], wb_, ALU.mult)
                    else:
                        P.tt("pool", r_[:, 0:kw], r_[:, 0:kw], wb_, ALU.mult)
                        P.tt("pool", sl_, sl_, r_[:, 0:kw], ALU.add)
                    if ch == nch - 1 and h == 3:
                        P.tt("pool", Sj[:, qb * 128:nk], Sj[:, qb * 128:nk], k.cbias, ALU.add)
                units.append(u_s)
        return units

    def thr(qb):
        j = qb % 2
        nk = (qb + 1) * 128
        Sj, mk = S[j], mb[j]
        b_ = bs[j]
        lo, hi, rng, tr_, d_, th = (b_[:, i:i + 1] for i in range(6))
        if qb >= 2:
            nd = qb * 128
            P.reduce("dve", hi, Sj[:, 0:nd], ALU.max)
            P.reduce("dve", lo, Sj[:, 0:nd], ALU.min)
            P.tt("dve", rng, hi, lo, ALU.subtract)
            P.ts("dve", halves[j], k.pow2, rng, None, op0=ALU.mult)
            P.tt("dve", tr_, lo, halves[j][:, 0:1], ALU.add)
            P.memset("dve", cn[j], 0.0)
            for it in range(NIT):
                c_ = cn[j][:, it:it + 1]
                h_ = halves[j][:, it:it + 1]
                P.ts("dve", mk[:, 0:nk], Sj[:, 0:nk], tr_, 0.0, op0=ALU.is_ge, op1=ALU.add, accum=c_)
                P.ts("dve", d_, c_, 255.5, 0.5, op0=ALU.is_ge, op1=ALU.subtract)
                P.stt("dve", tr_, d_, h_, tr_, ALU.mult, ALU.add)
            P.stt("dve", th, halves[j][:, NIT - 1:NIT], -0.5, tr_, ALU.mult, ALU.add)
        else:
            P.memset("dve", th, -1.0e29)
        P.ts("dve", mk[:, 0:nk], Sj[:, 0:nk], th, -30000.0, op0=ALU.is_lt, op1=ALU.mult)

    def att_units(qb):
        j = qb % 2
        mk = mb[j]
        units = []

        def pv(kc):
            v1 = V(V1.ap[:, kc, :], V1_b[kc])
            for h in range(8):
                P.mm(pO[h // 4][:, (h % 4) * 65:(h % 4) * 65 + 65], E[kc % 2][:, h * 128:(h + 1) * 128], v1,
                     start=(kc == 0 and h % 4 == 0), stop=(kc == qb and h % 4 == 3))

        for kc in range(qb + 1):
            def u(kc=kc):
                kTv = V(kT.ap[:, kc * 128:(kc + 1) * 128], kT_b[kc])
                for hh in range(2):
                    P.mm(pL[hh], kTv, qTb[qb % 3][:, hh * 512:(hh + 1) * 512], start=True, stop=False)
                    P.mm(pL[hh], mk[:, kc * 128:(kc + 1) * 128], id4, start=False, stop=True)
                    P.act(E[kc % 2][:, hh * 512:(hh + 1) * 512], pL[hh], AF.Exp, scale=0.125)
                if kc > 0:
                    pv(kc - 1)
                if kc == qb:
                    pv(kc)
            units.append(u)
        return units

    def norm(qb):
        j = qb % 2
        for hh in range(2):
            o3 = pO[hh][:, 0:260].re("p (h d) -> p h d", d=65)
            rc = rec[j][:, hh * 4:(hh + 1) * 4]
            P.generic("dve", lambda e, rc=rc, o3=o3: e.reciprocal(rc.ap, o3.ap[:, :, 64]), [o3], [rc])
            P.tt("dve", ya[j][:, hh * 256:(hh + 1) * 256].re("p (h d) -> p h d", d=64), o3[:, :, 0:64],
                 rc.un(2).bc([128, 4, 64]), ALU.mult)
        for c in range(4):
            P.tr(pT[:, c, :], ya[j][:, c * 128:(c + 1) * 128], k.identb)
        P.copy("act", yaT[j], pT[:, 0:4, :].re("p a b -> p (a b)"))
        P.dma("sp", V(k.YA.ap[qb].rearrange("p a b -> p (a b)"), k.Y_b["YA"][qb]), yaT[j], sb=yaT[j])

    def run_merged(a, b):
        import os
        if os.environ.get("A_INTERLEAVE") == "0":
            for f in b:
                f()
            for f in a:
                f()
            return
        ia = ib = 0
        while ia < len(a) or ib < len(b):
            if ib >= len(b) or (ia < len(a) and ia * len(b) <= ib * len(a)):
                a[ia]()
                ia += 1
            else:
                b[ib]()
                ib += 1

    run_merged(front_units(0), [])
    thr(0)
    run_merged(front_units(1), [])
    for i in range(NT):
        if i + 1 < NT:
            thr(i + 1)
        run_merged(front_units(i + 2) if i + 2 < NT else [], att_units(i))
        norm(i)
    P.end()


def phaseB(k, l):
    P = k.P
    P.begin("B%d" % l)
    w_in = k.w["w_in"][l]
    WB = P.sbuf("WB", [128, 8, 1552], BF16)
    load_w(P, WB[:, :, 0:1024], w_in[:, 964:1988], engs=("pool", "act", "dve"), nst=6)
    load_w(P, WB[:, :, 1024:1536], w_in[:, 2004:2516], engs=("pool", "act", "dve"))
    load_w(P, WB[:, :, 1536:1552], w_in[:, 1988:2004], engs=("pool", "act", "dve"))
    wa2 = P.sbuf("wa2", [16, 256], BF16)
    wa2f = P.sbuf("wa2f", [16, 256], F32)
    P.dma("sp", wa2f, k.w["gla_wa2"][l])
    P.copy("pool", wa2, wa2f)
    ba = P.sbuf("ba", [1, 256], BF16)
    baf = P.sbuf("baf", [1, 256], F32)
    P.dma("sp", baf, k.w["gla_ba"][l].re("(o n) -> o n", o=1))
    P.copy("pool", ba, baf)
    gng = P.sbuf("gng", [128, 128], F32)
    P.dma("sp", gng, brow(k.w["gla_norm_g"][l], 128))
    triub = P.sbuf("triub", [128, 128], BF16)
    P.copy("dve", triub, k.triu)
    St = P.sbuf("St", [64, 4, 128], F32)
    Sb = P.sbuf("Sb", [64, 2, 4, 128], BF16, nslots=2)
    glT = P.sbuf("glT", [16, 2, 128], BF16, nslots=2)
    e1 = P.sbuf("e1", [128, 2, 256], F32, nslots=2)
    lg = P.sbuf("lg", [128, 2, 256], F32, nslots=2)
    bsb = P.sbuf("bsb", [128, 2, 256], F32, nslots=2)
    dd = P.sbuf("dd", [128, 2, 256], F32, nslots=2)
    eq = P.sbuf("eq", [128, 2, 256], F32, nslots=2)
    ek = P.sbuf("ek", [128, 2, 256], F32, nslots=2)
    ekl = P.sbuf("ekl", [128, 2, 256], F32, nslots=2)
    qt = P.sbuf("qt", [128, 2, 256], BF16, nslots=2)
    kt = P.sbuf("kt", [128, 2, 256], BF16, nslots=2)
    kh = P.sbuf("kh", [128, 2, 256], BF16, nslots=2)
    vb = P.sbuf("vb", [128, 2, 512], BF16, nslots=2)
    dec = P.sbuf("dec", [64, 2, 4], F32, nslots=2)
    qkT = P.sbuf("qkT", [64, 2, 1024], BF16, nslots=2)
    att = P.sbuf("att", [128, 2, 512], BF16, nslots=2)
    ss = P.sbuf("ss", [128, 2, 4], F32, nslots=2)
    sl = P.sbuf("sl", [128, 2, 4], F32, nslots=2)
    ri = P.sbuf("ri", [128, 2, 4], F32, nslots=2)
    sq = P.sbuf("sq", [128, 128], BF16)
    yb = P.sbuf("yb", [128, 2, 512], F32, nslots=2)
    sr = P.sbuf("sr", [128, 2, 512], F32, nslots=2)
    yb2 = P.sbuf("yb2", [128, 2, 512], BF16, nslots=2)
    ybT = P.sbuf("ybT", [128, 2, 512], BF16, nslots=2)
    pQA = P.psum("pQA", [128, 512], F32)
    pV = P.psum("pV", [128, 512], F32)
    pR = P.psum("pR", [128, 512], F32)
    pZG = P.psum("pZG", [128, 512], F32)
    pCS = P.psum("pCS", [128, 512], F32)
    pTr = P.psum("pTr", [128, 8, 128], BF16)
    pOo = P.psum("pOo", [128, 512], F32)
    pX = P.psum("pX", [128, 512], F32)

    def front_units(t):
        j = t % 2
        h_ = hT(k, t)

        def u1():
            for dc in range(8):
                P.mm(pQA, h_[:, dc, :], WB[:, dc, 0:512], start=(dc == 0), stop=(dc == 7))
            for dc in range(8):
                P.mm(pV, h_[:, dc, :], WB[:, dc, 512:1024], start=(dc == 0), stop=(dc == 7))
            for dc in range(8):
                P.mm(pR, h_[:, dc, :], WB[:, dc, 1024:1536], start=(dc == 0), stop=(dc == 7))
            for dc in range(8):
                P.mm(pZG[0:16, 256:384], WB[:, dc, 1536:1552], h_[:, dc, :], start=(dc == 0), stop=(dc == 7))
            P.copy("act", glT[j], pZG[0:16, 256:384])

        def u2():
            P.mm(pZG[:, 0:256], glT[j], wa2, start=True, stop=False)
            P.mm(pZG[:, 0:256], k.onesb[0:1, :], ba, start=False, stop=True)
            P.act(e1[j], pZG[:, 0:256], AF.Exp, scale=-1.0)
            P.act(lg[j], e1[j], AF.Ln, bias=k.ones[:, 0:1], scale=1.0)

        def u3():
            P.mm(pCS[:, 0:256], k.triu, lg[j])
            P.mm(pCS[:, 256:512], k.ones, lg[j])
            for h in range(4):
                P.mm(pZG[0:64, 384 + h:385 + h], lg[j][:, h * 64:(h + 1) * 64], k.ones[:, 0:1])
            P.act(dec[j], pZG[0:64, 384:388], AF.Exp, scale=-1.0 / 16)
            P.act(eq[j], pCS[:, 0:256], AF.Exp, scale=-1.0 / 16)
            P.act(ek[j], pCS[:, 0:256], AF.Exp, scale=1.0 / 16)
            P.copy("act", bsb[j], pCS[:, 0:256])
            P.tt("dve", dd[j], pCS[:, 256:512], bsb[j], ALU.subtract)
            P.act(ekl[j], dd[j], AF.Exp, scale=-1.0 / 16)
            P.stt("dve", qt[j], pQA[:, 0:256], 0.125, eq[j], ALU.mult, ALU.mult)
            P.tt("dve", kt[j], pQA[:, 256:512], ek[j], ALU.mult)
            P.tt("dve", kh[j], pQA[:, 256:512], ekl[j], ALU.mult)
            P.copy("act", vb[j], pV)
            P.act(sr[j], pR, AF.Silu)
        return [u1, u2, u3]

    def back_units(t):
        j = t % 2

        def v1():
            for h in range(4):
                P.tr(pTr[0:64, h, :], qt[j][:, h * 64:(h + 1) * 64], k.identb)
                P.tr(pTr[0:64, 4 + h, :], kt[j][:, h * 64:(h + 1) * 64], k.identb)
            P.copy("act", qkT[j], pTr[0:64, :, :].re("p a b -> p (a b)"))

        def v2():
            for h in range(4):
                P.mm(pX[:, h * 128:(h + 1) * 128], qkT[j][:, (4 + h) * 128:(5 + h) * 128], qkT[j][:, h * 128:(h + 1) * 128])
            P.tt("dve", att[j].re("p (h t) -> p h t", h=4), pX.re("p (h t) -> p h t", h=4),
                 triub.un(1).bc([128, 4, 128]), ALU.mult)

        def v3():
            Sprev = Sb[(t + 1) % 2]
            for h in range(4):
                P.mm(pOo[:, h * 128:(h + 1) * 128], att[j][:, h * 128:(h + 1) * 128], vb[j][:, h * 128:(h + 1) * 128],
                     start=True, stop=(t == 0))
                if t > 0:
                    P.mm(pOo[:, h * 128:(h + 1) * 128], qkT[j][:, h * 128:(h + 1) * 128], Sprev[:, h, :],
                         start=False, stop=True)
            if t < NT - 1:
                pSt = pX[0:64, :]
                for h in range(4):
                    P.mm(pSt[:, h * 128:(h + 1) * 128], kh[j][:, h * 64:(h + 1) * 64], vb[j][:, h * 128:(h + 1) * 128])
                if t == 0:
                    P.copy("dve", St, pSt.re("p (h v) -> p h v", h=4))
                else:
                    for h in range(4):
                        P.stt("dve", St[:, h, :], St[:, h, :], dec[j][:, h:h + 1], pSt[:, h * 128:(h + 1) * 128],
                              ALU.mult, ALU.add)
                P.copy("act", Sb[j], St)

        def v4():
            P.memset("pool", ss[j], 0.0)
            for h in range(4):
                P.act(sq, pOo[:, h * 128:(h + 1) * 128], AF.Square, accum=ss[j][:, h:h + 1])
            P.act(sl[j], ss[j], AF.Ln, bias=k.eps6, scale=1.0 / 128)
            P.act(ri[j], sl[j], AF.Exp, scale=-0.5)
            P.tt("dve", yb[j].re("p (h v) -> p h v", h=4), pOo.re("p (h v) -> p h v", h=4),
                 ri[j].un(2).bc([128, 4, 128]), ALU.mult)
            P.tt("pool", yb[j].re("p (h v) -> p h v", h=4), yb[j].re("p (h v) -> p h v", h=4),
                 gng.un(1).bc([128, 4, 128]), ALU.mult)
            P.tt("pool", yb2[j], yb[j], sr[j], ALU.mult)

        def v5():
            for c in range(4):
                P.tr(pTr[:, c, :], yb2[j][:, c * 128:(c + 1) * 128], k.identb)
            P.copy("act", ybT[j], pTr[:, 0:4, :].re("p a b -> p (a b)"))
            P.dma("sp", V(k.YB.ap[t].rearrange("p a b -> p (a b)"), k.Y_b["YB"][t]), ybT[j], sb=ybT[j])
        return [v1, v2, v3, v4, v5]

    def run_merged(a, b):
        ia = ib = 0
        while ia < len(a) or ib < len(b):
            if ib >= len(b) or (ia < len(a) and ia * len(b) <= ib * len(a)):
                a[ia]()
                ia += 1
            else:
                b[ib]()
                ib += 1

    run_merged(front_units(0), [])
    for t in range(NT):
        run_merged(front_units(t + 1) if t + 1 < NT else [], back_units(t))
    P.end()


def phaseC(k, l):
    P = k.P
    P.begin("C%d" % l)
    w_in = k.w["w_in"][l]
    WC = P.sbuf("WC", [128, 8, 1024], BF16)
    load_w(P, WC, w_in[:, 2516:3540], engs=("pool", "act", "dve"), nst=6)
    gg = P.sbuf("gg", [128, 512], F32)
    gb = P.sbuf("gb", [128, 512], F32)
    P.dma("sp", gg, brow(k.w["gm_ln_g"][l], 512))
    P.dma("sp", gb, brow(k.w["gm_ln_b"][l], 512))
    wsf = P.sbuf("wsf", [128, 4, 128], F32)
    P.dma("sp", wsf, k.w["gm_ws"][l].re("g t s -> t g s"))
    bsT = P.sbuf("bsT", [128, 4], F32)
    P.dma("sp", bsT, k.w["gm_bs"][l].re("g t -> t g"), allow_slow_non_contiguous=True)
    WsT = P.sbuf("WsT", [128, 4, 128], BF16)
    pW = P.psum("pW", [128, 4, 128], F32)
    for g in range(4):
        P.tr(pW[:, g, :], wsf[:, g, :], k.ident)
    P.tt("dve", WsT, pW, k.triu.un(1).bc([128, 4, 128]), ALU.mult)
    pU = P.psum("pU", [128, 2, 512], F32, nslots=2)
    pVv = P.psum("pVv", [128, 2, 512], F32, nslots=2)
    pM = P.psum("pM", [128, 512], F32)
    pTr = P.psum("pTr", [128, 4, 128], BF16)
    u = P.sbuf("u", [128, 2, 512], F32, nslots=2)
    vv = P.sbuf("vv", [128, 2, 512], F32, nslots=2)
    vn = P.sbuf("vn", [128, 2, 512], F32, nslots=2)
    vnb = P.sbuf("vnb", [128, 2, 512], BF16, nslots=2)
    mx = P.sbuf("mx", [128, 2, 512], F32, nslots=2)
    yc = P.sbuf("yc", [128, 2, 512], BF16, nslots=2)
    ycT = P.sbuf("ycT", [128, 2, 512], BF16, nslots=2)
    ln = LN(k, "lnc", 512, k.eps5)

    def s1(t):
        j = t % 2
        h_ = hT(k, t)
        for dc in range(8):
            P.mm(pU[j], h_[:, dc, :], WC[:, dc, 0:512], start=(dc == 0), stop=(dc == 7))
        for dc in range(8):
            P.mm(pVv[j], h_[:, dc, :], WC[:, dc, 512:1024], start=(dc == 0), stop=(dc == 7))
        P.act(u[j], pU[j], AF.Gelu_apprx_tanh)
        P.act(vv[j], pVv[j], AF.Gelu_apprx_tanh)
        ln(vv[j], vn[j], gg, None)
        P.tt("pool", vnb[j], vn[j], gb, ALU.add)

    def s2(t):
        j = t % 2
        for g in range(4):
            P.mm(pM[:, g * 128:(g + 1) * 128], WsT[:, g, :], vnb[j][:, g * 128:(g + 1) * 128])
        P.tt("dve", mx[j].re("p (g d) -> p g d", g=4), pM.re("p (g d) -> p g d", g=4),
             bsT.un(2).bc([128, 4, 128]), ALU.add)
        P.tt("pool", yc[j], mx[j], u[j], ALU.mult)

    def s3(t):
        j = t % 2
        for c in range(4):
            P.tr(pTr[:, c, :], yc[j][:, c * 128:(c + 1) * 128], k.identb)
        P.copy("act", ycT[j], pTr.re("p a b -> p (a b)"))
        P.dma("sp", V(k.YC.ap[t].rearrange("p a b -> p (a b)"), k.Y_b["YC"][t]), ycT[j], sb=ycT[j])

    s1(0)
    s1(1)
    s2(0)
    for t in range(NT):
        if t + 2 < NT:
            s1(t + 2)
        if t + 1 < NT:
            s2(t + 1)
        s3(t)
    P.end()


def phaseM(k, l):
    phaseM1(k, l)
    if getattr(k, "stop_m1", False):
        return
    phaseM2(k, l)


def phaseM1(k, l):
    P = k.P
    P.begin("M1_%d" % l)
    w_in = k.w["w_in"][l]
    WG = P.sbuf("WG", [128, 8, 3072], BF16)
    load_w(P, WG, w_in[:, 3540:6612], engs=("pool", "act", "dve"), nst=5)
    WBR = P.sbuf("WBR", [128, 3, 4, 1024], BF16)
    for i, n in enumerate(("w_branch_a", "w_branch_b", "w_branch_c")):
        load_w(P, WBR[:, i], k.w[n][l], engs=("pool", "act", "dve"))
    yT = {n: P.sbuf("m" + n, [128, 2, 512], BF16, nslots=2) for n in ("YA", "YB", "YC")}
    sg = P.sbuf("sg", [128, 2, 512], F32, nslots=2)
    tmp = P.sbuf("tmp", [128, 2, 512], F32, nslots=2)
    hm = P.sbuf("hm", [128, 2, D], F32, nslots=2)
    hmb = P.sbuf("hmb", [128, 2, D], BF16, nslots=2)
    hmT = P.sbuf("hmT", [128, 2, D], BF16, nslots=2)
    pG = P.psum("pG", [128, 2, 512], F32, nslots=2)
    pP = P.psum("pP", [128, 2, 512], F32, nslots=2)
    pTb = P.psum("pTb", [128, 2, 8, 128], BF16, nslots=2)
    cnt = 0

    def prefetch(t):
        for n in ("YA", "YB", "YC"):
            P.dma("sp", yT[n][t % 2], V(getattr(k, n).ap[t].rearrange("p a b -> p (a b)"), k.Y_b[n][t]))

    def finish(t):
        j = t % 2
        P.copy("pool", hmb[j], hm[j])
        for c in range(8):
            P.tr(pTb[j][:, c, :], hmb[j][:, c * 128:(c + 1) * 128], k.identb)
        P.copy("act", hmT[j], pTb[j].re("p a b -> p (a b)"))
        P.dma("sp", V(k.HM.ap[t], k.HM_b[t]), hmT[j], sb=hmT[j])

    prefetch(0)
    for t in range(NT):
        j = t % 2
        h_ = hT(k, t)
        if t + 1 < NT:
            prefetch(t + 1)
        for xi, n in enumerate(("YA", "YB", "YC")):
            for nb in range(2):
                if t > 0 and xi == 1 and nb == 0:
                    finish(t - 1)
                pg, pp = pG[cnt % 2], pP[cnt % 2]
                s_, t_ = sg[cnt % 2], tmp[cnt % 2]
                cnt += 1
                c0 = xi * 1024 + nb * 512
                for dc in range(8):
                    P.mm(pg, h_[:, dc, :], WG[:, dc, c0:c0 + 512], start=(dc == 0), stop=(dc == 7))
                for c in range(4):
                    P.mm(pp, yT[n][j][:, c * 128:(c + 1) * 128], WBR[:, xi, c, nb * 512:(nb + 1) * 512],
                         start=(c == 0), stop=(c == 3))
                P.act(s_, pg, AF.Sigmoid)
                if xi == 0:
                    P.tt("dve", hm[j][:, nb * 512:(nb + 1) * 512], pp, s_, ALU.mult)
                else:
                    P.tt("dve", t_, pp, s_, ALU.mult)
                    P.tt("pool", hm[j][:, nb * 512:(nb + 1) * 512], hm[j][:, nb * 512:(nb + 1) * 512], t_, ALU.add)
    finish(NT - 1)
    P.end()


def phaseM2(k, l):
    P = k.P
    P.begin("M2_%d" % l)
    WO = P.sbuf("WO", [128, 8, 1024], BF16)
    load_w(P, WO, k.w["w_out"][l], engs=("pool", "act", "dve"), nst=6)
    WR = P.sbuf("WR", [128, 8, 36], F32)
    for c in range(8):
        P.dma("sp", WR[:, c, :], k.w["wr_cat"][l][c * 128:(c + 1) * 128, :], sb=WR)
    rbias = P.sbuf("rbias", [128, 36], F32)
    P.dma("sp", rbias, brow(k.w["br_cat"][l], 36))
    g1 = P.sbuf("g1", [128, D], F32)
    b1 = P.sbuf("b1", [128, D], F32)
    P.dma("sp", g1, brow(k.w["ln1_g"][l], D))
    P.dma("sp", b1, brow(k.w["ln1_b"][l], D))
    hmT = P.sbuf("hmT", [128, 3, D], BF16, nslots=3)
    hres_t = P.sbuf("hres_t", [128, 3, D], F32, nslots=3)
    r = P.sbuf("r", [128, 2, D], F32, nslots=2)
    h1 = P.sbuf("h1", [128, 2, D], F32, nslots=2)
    hTf = P.sbuf("hTf", [128, 2, D], F32, nslots=2)
    rt_ = P.sbuf("rt_", [128, 2, 128], F32, nslots=2)
    ln = LN(k, "ln1", D, k.eps5)
    pX = P.psum("pX", [128, 2, 512], F32, nslots=2)
    pT = P.psum("pT", [128, 2, 8, 128], F32, nslots=2)
    pRt = P.psum("pRt", [128, 512], F32)

    def prefetch(t):
        P.dma("sp", hmT[t % 3], V(k.HM.ap[t], k.HM_b[t]))
        P.dma("sp", hres_t[t % 3], hres_tile(k, t))

    def stage1(t):
        j = t % 2
        for nb in range(2):
            for dc in range(8):
                P.mm(pX[nb], hmT[t % 3][:, dc * 128:(dc + 1) * 128], WO[:, dc, nb * 512:(nb + 1) * 512],
                     start=(dc == 0), stop=(dc == 7))
            P.stt("dve", r[j][:, nb * 512:(nb + 1) * 512], hres_t[t % 3][:, nb * 512:(nb + 1) * 512], float(DN_ALPHA),
                  pX[nb], ALU.mult, ALU.add)
        ln.part1(r[j], h1[j])

    def stage2(t):
        j = t % 2
        ln.part2(h1[j], g1, b1, "pool", "dve")
        P.dma("sp", hres_tile(k, t), h1[j], sb=h1[j])
        for c in range(8):
            P.tr(pT[j][:, c, :], h1[j][:, c * 128:(c + 1) * 128], k.ident)
        for hh in range(2):
            P.copy("act", hTf[j][:, hh * 512:(hh + 1) * 512], pT[j][:, hh * 4:(hh + 1) * 4, :].re("p a b -> p (a b)"))
        for hh in range(2):
            P.copy("act", hT(k, t)[:, hh * 4:(hh + 1) * 4, :], pT[j][:, hh * 4:(hh + 1) * 4, :])

    def stage3(t):
        router(k, t, hTf[t % 2], WR, rbias, pRt, rt_[t % 2])

    prefetch(0)
    prefetch(1)
    stage1(0)
    for t in range(NT):
        if t + 1 < NT:
            if t + 2 < NT:
                prefetch(t + 2)
            stage1(t + 1)
        stage2(t)
        if t >= 1:
            stage3(t - 1)
    stage3(NT - 1)
    P.end()


def router(k, t, hTf, WR, rbias, ps, s):
    P = k.P
    for dc in range(8):
        P.mm(ps[:, 0:36], hTf[:, dc * 128:(dc + 1) * 128], WR[:, dc, :], start=(dc == 0), stop=(dc == 7))
    lg = s[:, 0:36]
    P.tt("dve", lg, ps[:, 0:36], rbias, ALU.add)
    gl, el = s[:, 0:4], s[:, 4:36]
    gmax, gsum, gp, d21, e21, den, w1, w2 = (s[:, 40 + i:41 + i] for i in range(8))
    ge, oh, pen = s[:, 48:52], s[:, 52:56], s[:, 56:60]
    top8 = s[:, 60:68]
    elm = s[:, 68:100]
    P.reduce("dve", gmax, gl, ALU.max)
    P.ts("dve", ge, gl, gmax, None, op0=ALU.subtract)
    P.memset("dve", gsum, 0.0)
    P.act(ge, ge, AF.Exp, accum=gsum)
    P.generic("dve", lambda e: e.reciprocal(gp.ap, gsum.ap), [gsum], [gp])
    P.ts("dve", pen, gl, gmax, float(NEG), op0=ALU.is_lt, op1=ALU.mult)
    P.tt("dve", elm.re("p (g e) -> p g e", g=4), el.re("p (g e) -> p g e", g=4), pen.un(2).bc([128, 4, 8]), ALU.add)
    P.generic("dve", lambda e: e.max(top8.ap, elm.ap), [elm], [top8])
    m1, m2 = top8[:, 0:1], top8[:, 1:2]
    P.tt("dve", d21, m2, m1, ALU.subtract)
    P.act(e21, d21, AF.Exp)
    P.ts("dve", den, e21, 1.0, None, op0=ALU.add)
    P.generic("dve", lambda e: e.reciprocal(den.ap, den.ap), [den], [den])
    P.tt("dve", w1, den, gp, ALU.mult)
    P.tt("dve", w2, w1, e21, ALU.mult)
    cm = k.COMB[:, t, :]
    P.ts("dve", cm, elm, m1, w1, op0=ALU.is_equal, op1=ALU.mult)
    P.ts("dve", elm, elm, m2, w2, op0=ALU.is_equal, op1=ALU.mult)
    P.tt("dve", cm, cm, elm, ALU.add)


def phaseE(k, l, last):
    P = k.P
    P.begin("E%d" % l)
    HALF = NT // 2
    g2 = P.sbuf("g2", [128, D], F32)
    b2 = P.sbuf("b2", [128, D], F32)
    P.dma("sp", g2, brow(k.w["ln2_g"][l], D))
    P.dma("sp", b2, brow(k.w["ln2_b"][l], D))
    acc = P.sbuf("acc", [128, HALF, D], F32, nslots=HALF)
    import os
    NWS = 3 if USE_SWDGE else 2
    wg = P.sbuf("wg", [128, NWS, 8, 256], BF16, nslots=NWS)
    wu = P.sbuf("wu", [128, NWS, 8, 256], BF16, nslots=NWS)
    wd = P.sbuf("wd", [128, NWS, 2, 1024], BF16, nslots=NWS)
    sgt = P.sbuf("sgt", [128, 2, 512], BF16, nslots=2)
    hid = P.sbuf("hid", [128, 4, 512], BF16, nslots=4)
    h1t = P.sbuf("h1t", [128, 3, D], F32, nslots=3)
    ln = LN(k, "ln2", D, k.eps5)
    pGt = P.psum("pGt", [128, 2, 512], F32, nslots=2)
    pUp = P.psum("pUp", [128, 2, 512], F32, nslots=2)
    pOd = P.psum("pOd", [128, 4, 512], F32, nslots=4)
    pT = None
    w_gate, w_up, w_down = k.w["w_gate"][l], k.w["w_up"][l], k.w["w_down"][l]
    cg = [0]
    co = [0]

    def gate_up(s, tg):
        hids = []
        for fb in range(2):
            pg, pu = pGt[cg[0] % 2], pUp[cg[0] % 2]
            s_ = sgt[cg[0] % 2]
            hd = hid[cg[0] % 4]
            cg[0] += 1
            for dc in range(8):
                P.mm(pg, wg[s][:, dc, fb * 128:(fb + 1) * 128], hT_span(k, dc, tg, 4), start=(dc == 0), stop=(dc == 7))
            for dc in range(8):
                P.mm(pu, wu[s][:, dc, fb * 128:(fb + 1) * 128], hT_span(k, dc, tg, 4), start=(dc == 0), stop=(dc == 7))
            P.act(s_, pg, AF.Silu)
            P.tt("dve", hd, pu, s_, ALU.mult)
            hids.append(hd)
        return hids

    def down(s, e, tg, t0, hids):
        for ti in range(4):
            t = tg + ti
            pos = []
            for nb in range(2):
                pos.append(pOd[co[0] % 4])
                co[0] += 1
            if ti == 0:
                order = [(0, 0), (1, 0), (0, 1), (1, 1)]
            else:
                order = [(0, 0), (0, 1), (1, 0), (1, 1)]
            for nb, fb in order:
                P.mm(pos[nb], hids[fb][:, ti * 128:(ti + 1) * 128], wd[s][:, fb, nb * 512:(nb + 1) * 512],
                     start=(fb == 0), stop=(fb == 1))
            for nb in range(2):
                a_ = acc[t - t0][:, nb * 512:(nb + 1) * 512]
                cw = k.COMB[:, t, e:e + 1]
                if e == 0:
                    P.ts("dve", a_, pos[nb], cw, None, op0=ALU.mult)
                else:
                    P.stt("dve", a_, pos[nb], cw, a_, ALU.mult, ALU.add)

    for half in range(2):
        t0 = half * HALF
        for e in range(32):
            s = (half * 32 + e) % NWS
            load_w(P, wg[s], w_gate[e], engs=("pool", "act"))
            load_w(P, wu[s], w_up[e], engs=("pool", "act"))
            load_w(P, wd[s], w_down[e], engs=("pool", "act"))
            for grp in range(HALF // 4):
                tg = t0 + grp * 4
                hids = gate_up(s, tg)
                down(s, e, tg, t0, hids)
        def tail1(ti):
            t = t0 + ti
            a_ = acc[ti]
            P.stt("dve", a_, h1t[ti % 3], float(DN_ALPHA), a_, ALU.mult, ALU.add)
            ln.part1(a_, a_)

        def tail1b(ti):
            t = t0 + ti
            a_ = acc[ti]
            ln.part2(a_, g2, b2, "pool", "dve")
            if last:
                P.dma("sp", V(k.out.ap[t * 128:(t + 1) * 128, :], Buf("o%d" % t)), a_, sb=a_)
            else:
                P.dma("sp", hres_tile(k, t), a_, sb=a_)

        def tail2(ti):
            t = t0 + ti
            a_ = acc[ti]
            pt = (pOd[0], pOd[1]) if ti % 2 == 0 else (pOd[2], pOd[3])
            for c in range(8):
                P.tr(pt[c // 4][:, (c % 4) * 128:(c % 4 + 1) * 128], a_[:, c * 128:(c + 1) * 128], k.ident)
            for hh in range(2):
                P.copy("act", hT(k, t)[:, hh * 4:(hh + 1) * 4, :], pt[hh].re("p (a b) -> p a b", a=4))

        P.dma("sp", h1t[0], hres_tile(k, t0))
        P.dma("sp", h1t[1], hres_tile(k, t0 + 1))
        tail1(0)
        for ti in range(HALF):
            if ti + 2 < HALF:
                P.dma("sp", h1t[(ti + 2) % 3], hres_tile(k, t0 + ti + 2))
            if ti + 1 < HALF:
                tail1(ti + 1)
            tail1b(ti)
            if not last:
                tail2(ti)
    P.end()


_CACHE = {}


def make_in_maps(inputs, n_cores=8):
    consts = host_consts()
    inputs = dict(inputs)
    inputs["wr_cat"] = np.concatenate([np.asarray(inputs["w_rg"]), np.asarray(inputs["w_re"])], axis=-1)
    inputs["br_cat"] = np.concatenate([np.asarray(inputs["b_rg"]), np.asarray(inputs["b_re"])], axis=-1)
    shared = {n: np.ascontiguousarray(np.asarray(inputs[n], dtype=np.float32)) for n, _ in WNAMES}
    shared.update(consts)
    maps = []
    x = np.asarray(inputs["x"], dtype=np.float32)
    pos = np.asarray(inputs["positions"]).astype(np.int32)
    for b in range(n_cores):
        m = dict(shared)
        m["x"] = np.ascontiguousarray(x[b])
        m["pos"] = np.ascontiguousarray(pos[b].reshape(NT, 128).T)
        maps.append(m)
    return maps


def kernel(**inputs):
    if "nc" not in _CACHE:
        _CACHE["nc"] = build()[0]
    nc = _CACHE["nc"]
    maps = make_in_maps(inputs, 8)
    res = run_bass_kernel_spmd(nc, maps, core_ids=list(range(8)))
    return np.stack([np.asarray(r["out"], dtype=np.float32) for r in res.results], axis=0)
```

```python
import numpy as np
from contextlib import ExitStack
import concourse.bass as bass
import concourse.mybir as mybir
from concourse.bass_utils import run_bass_kernel_spmd

F32 = mybir.dt.float32
BF16 = mybir.dt.bfloat16
I32 = mybir.dt.int32
AF = mybir.ActivationFunctionType
ALU = mybir.AluOpType
AX = mybir.AxisListType


class Buf:
    __slots__ = ("name", "last_write", "readers", "dstream")

    def __init__(self, name):
        self.name = name
        self.last_write = None
        self.readers = []
        self.dstream = None


class V:
    __slots__ = ("ap", "buf")

    def __init__(self, ap, buf):
        self.ap = ap
        self.buf = buf

    def __getitem__(self, idx):
        return V(self.ap[idx], self.buf)

    def re(self, pat, **kw):
        return V(self.ap.rearrange(pat, **kw), self.buf)

    def bc(self, shape):
        return V(self.ap.to_broadcast(list(shape)), self.buf)

    def un(self, axis):
        return V(self.ap.unsqueeze(axis), self.buf)

    def wb(self, bufs):
        return V(self.ap, bufs)


class Op:
    __slots__ = ("eng", "fn", "deps", "is_dma", "stream", "signal", "count", "tag")


class Stream:
    def __init__(self, name, inc):
        self.name = name
        self.inc = inc
        self.total = 0
        self.sem = None


NDSEM = 90
STRICT = True
import os
USE_SWDGE = bool(os.environ.get("USE_SWDGE"))
ENGS = ("pe", "act", "dve", "pool", "sp")
ENGOBJ = {"pe": "tensor", "act": "scalar", "dve": "vector", "pool": "gpsimd", "sp": "sync"}


def _bufs(vs):
    out = []
    for v in vs:
        b = v.buf if isinstance(v, V) else v
        if isinstance(b, (tuple, list)):
            for x in b:
                if x not in out:
                    out.append(x)
        elif b not in out:
            out.append(b)
    return out


class Prog:
    def __init__(self, nc):
        self.nc = nc
        self.ges = ExitStack()
        self.estream = {e: Stream("s_" + e, 1) for e in ENGS}
        for e in ENGS:
            self.estream[e].sem = self.ges.enter_context(nc.semaphore("s_" + e))
        self.dsem_pool = [self.ges.enter_context(nc.semaphore("dsem%d" % i)) for i in range(NDSEM)]
        self.dsem_next = 0
        self.dsem_base = [0] * NDSEM
        self.nphase = 0
        self.tot_ops = 0
        self.tot_waits = 0
        self.pes = None

    def _es(self, glob):
        return self.ges if glob else self.pes

    def sbuf(self, name, shape, dtype, nslots=None, glob=False):
        if not glob:
            name = self.pname + "_" + name
        t = self._es(glob).enter_context(self.nc.sbuf_tensor(name, list(shape), dtype))
        if nslots is None:
            return V(t[:], Buf(name))
        return [V(t[:, i], Buf(f"{name}{i}")) for i in range(nslots)]

    def psum(self, name, shape, dtype=F32, nslots=None):
        name = self.pname + "_" + name
        t = self.pes.enter_context(self.nc.psum_tensor(name, list(shape), dtype))
        if nslots is None:
            return V(t[:], Buf(name))
        return [V(t[:, i], Buf(f"{name}{i}")) for i in range(nslots)]

    def dram(self, name, shape, dtype, kind="Internal"):
        t = self.nc.dram_tensor(name, list(shape), dtype, kind=kind)
        return V(t.ap(), Buf(name))

    def begin(self, name):
        self.pname = name
        self.pes = ExitStack()
        self.ops = {e: [] for e in ENGS}
        self.all_ops = []
        self.dstreams = []
        self.touched = []

    def _rec(self, eng, fn, reads, writes, is_dma=False, dbuf=None, tag=""):
        op = Op()
        op.eng = eng
        op.fn = fn
        op.is_dma = is_dma
        op.signal = is_dma
        op.count = 0
        op.tag = tag
        if is_dma:
            b = _bufs([dbuf])[0]
            if b.dstream is None:
                b.dstream = Stream("d%d_%s" % (self.nphase, b.name), 16)
                self.dstreams.append(b.dstream)
                self.touched.append(b)
            op.stream = b.dstream
        else:
            op.stream = self.estream[eng]
        rb = _bufs(reads)
        wb = _bufs(writes)
        deps = []
        for b in rb:
            lw = b.last_write
            if lw is not None:
                deps.append((lw, "raw"))
        for b in wb:
            lw = b.last_write
            if lw is not None:
                deps.append((lw, "waw"))
            for r in b.readers:
                deps.append((r, "war"))
        fdeps = []
        for d, kind in deps:
            if d is op:
                continue
            if (not d.is_dma) and (not is_dma) and d.eng == eng:
                if eng == "pe" or (kind != "raw" and not STRICT):
                    continue
            if d.is_dma and is_dma and d.stream is op.stream and kind == "waw":
                continue
            fdeps.append(d)
        op.deps = fdeps
        for b in rb:
            b.readers.append(op)
            self.touched.append(b)
        for b in wb:
            b.last_write = op
            b.readers = []
            self.touched.append(b)
        self.ops[eng].append(op)
        self.all_ops.append(op)
        return op

    def mm(self, out, lhsT, rhs, start=True, stop=True, **kw):
        return self._rec("pe", lambda e: e.matmul(out.ap, lhsT.ap, rhs.ap, start=start, stop=stop, **kw),
                         [lhsT, rhs] + ([] if start else [out]), [out])

    def tr(self, out, in_, ident):
        return self._rec("pe", lambda e: e.transpose(out.ap, in_.ap, ident.ap), [in_, ident], [out])

    def act(self, out, in_, func, bias=None, scale=None, accum=None):
        reads = [in_]
        kw = {}
        if bias is not None:
            if isinstance(bias, V):
                reads.append(bias)
                kw["bias"] = bias.ap
            else:
                kw["bias"] = bias
        if scale is not None:
            if isinstance(scale, V):
                reads.append(scale)
                kw["scale"] = scale.ap
            else:
                kw["scale"] = scale
        writes = [out]
        if accum is not None:
            kw["accum_out"] = accum.ap
            writes.append(accum)
        return self._rec("act", lambda e: e.activation(out.ap, in_.ap, func, **kw), reads, writes)

    def tt(self, eng, out, a, b, op):
        return self._rec(eng, lambda e: e.tensor_tensor(out.ap, a.ap, b.ap, op), [a, b], [out])

    def ts(self, eng, out, a, s1, s2=None, op0=ALU.mult, op1=None, accum=None):
        reads = [a]
        s1a = s1.ap if isinstance(s1, V) else s1
        s2a = s2.ap if isinstance(s2, V) else s2
        if isinstance(s1, V):
            reads.append(s1)
        if isinstance(s2, V):
            reads.append(s2)
        writes = [out]
        kw = {}
        if op1 is not None:
            kw["op1"] = op1
        if accum is not None:
            kw["accum_out"] = accum.ap
            writes.append(accum)
        return self._rec(eng, lambda e: e.tensor_scalar(out.ap, a.ap, s1a, s2a, op0, **kw), reads, writes)

    def stt(self, eng, out, a, s, b, op0, op1):
        reads = [a, b]
        sa = s.ap if isinstance(s, V) else s
        if isinstance(s, V):
            reads.append(s)
        return self._rec(eng, lambda e: e.scalar_tensor_tensor(out.ap, a.ap, sa, b.ap, op0, op1), reads, [out])

    def copy(self, eng, out, in_):
        if eng == "act":
            return self._rec("act", lambda e: e.copy(out.ap, in_.ap), [in_], [out])
        return self._rec(eng, lambda e: e.tensor_copy(out.ap, in_.ap), [in_], [out])

    def memset(self, eng, out, val):
        return self._rec(eng, lambda e: e.memset(out.ap, val), [], [out])

    def reduce(self, eng, out, in_, op, axis=AX.X):
        return self._rec(eng, lambda e: e.tensor_reduce(out.ap, in_.ap, axis, op), [in_], [out])

    def generic(self, eng, fn, reads, writes):
        return self._rec(eng, fn, reads, writes)

    def dma(self, q, out, in_, sb=None, **kw):
        if sb is None:
            sb = out
        return self._rec(q, lambda e: e.dma_start(out.ap, in_.ap, **kw), [in_], [out], is_dma=True, dbuf=sb)

    def end(self):
        nc = self.nc
        for op in self.all_ops:
            for d in op.deps:
                d.signal = True
        for e in ENGS:
            for op in reversed(self.ops[e]):
                if not op.is_dma:
                    op.signal = True
                    break
        start_tot = {e: self.estream[e].total for e in ENGS}
        assert len(self.dstreams) <= NDSEM, len(self.dstreams)
        for s in self.dstreams:
            s.idx = self.dsem_next % NDSEM
            self.dsem_next += 1
            s.sem = self.dsem_pool[s.idx]
            s.total = self.dsem_base[s.idx]
            s.base = s.total
        for e in ENGS:
            for op in self.ops[e]:
                if op.signal:
                    st = op.stream
                    st.total += st.inc
                    op.count = st.total
        end_tot = {e: self.estream[e].total for e in ENGS}
        nwaits = 0
        with nc.Block() as block:
            def make(ename):
                def body(e):
                    nonlocal nwaits
                    seen = {}
                    for x in ENGS:
                        if x != ename and start_tot[x] > 0:
                            e.wait_ge(self.estream[x].sem, start_tot[x])
                        seen[self.estream[x]] = start_tot[x]
                    for op in self.ops[ename]:
                        need = {}
                        for d in op.deps:
                            st = d.stream
                            if d.count > seen.get(st, 0) and d.count > need.get(st, 0):
                                need[st] = d.count
                        for st, c in need.items():
                            e.wait_ge(st.sem, c)
                            seen[st] = c
                            nwaits += 1
                        ins = op.fn(e)
                        if op.signal:
                            ins.then_inc(op.stream.sem, op.stream.inc)
                    if ename == "sp":
                        for s in self.dstreams:
                            if s.total > s.base:
                                e.wait_ge(s.sem, s.total)
                        for x in ENGS:
                            if x != "sp" and end_tot[x] > start_tot[x]:
                                e.wait_ge(self.estream[x].sem, end_tot[x])
                        st = self.estream["sp"]
                        st.total += 1
                        e.sem_inc(st.sem, 1)
                return body
            for ename in ENGS:
                getattr(block, ENGOBJ[ename])(make(ename))
        for s in self.dstreams:
            self.dsem_base[s.idx] = s.total
        for b in self.touched:
            b.last_write = None
            b.readers = []
            b.dstream = None
        self.tot_ops += len(self.all_ops)
        self.tot_waits += nwaits
        self.nphase += 1
        print('phase', self.pname, 'ops', len(self.all_ops), 'waits', nwaits, 'totals', {e: self.estream[e].total for e in ENGS}, 'ndma_streams', len(self.dstreams), 'sbuf_free', self.nc.sbuf_bytes_remaining, flush=True)
        self.pes.close()
        self.pes = None

    def finish(self):
        self.ges.close()

D = 1024
SEQ = 4096
NT = SEQ // 128
DEPTH = 2
N_IN = 6612
DN_ALPHA = (2 * DEPTH) ** 0.25
NIT = 18
NEG = -1.0e30
TWO_PI = 6.283185307179586
C1 = 6.28125
C2 = TWO_PI - C1

WNAMES = [("ln_in_g", [D]), ("ln_in_b", [D]), ("w_in", [DEPTH, D, N_IN]), ("idx_k_g", [DEPTH, 64]),
          ("gla_wa2", [DEPTH, 16, 256]), ("gla_ba", [DEPTH, 256]), ("gla_norm_g", [DEPTH, 128]),
          ("gm_ln_g", [DEPTH, 512]), ("gm_ln_b", [DEPTH, 512]), ("gm_ws", [DEPTH, 4, 128, 128]),
          ("gm_bs", [DEPTH, 4, 128]), ("w_branch_a", [DEPTH, 512, D]), ("w_branch_b", [DEPTH, 512, D]),
          ("w_branch_c", [DEPTH, 512, D]), ("w_out", [DEPTH, D, D]), ("ln1_g", [DEPTH, D]), ("ln1_b", [DEPTH, D]),
          ("wr_cat", [DEPTH, D, 36]), ("br_cat", [DEPTH, 36]),
          ("w_gate", [DEPTH, 32, D, 256]), ("w_up", [DEPTH, 32, D, 256]), ("w_down", [DEPTH, 32, 256, D]),
          ("ln2_g", [DEPTH, D]), ("ln2_b", [DEPTH, D])]


def host_consts():
    ident = np.eye(128, dtype=np.float32)
    triu = np.triu(np.ones((128, 128), np.float32))
    cbias = np.where(np.arange(128)[None, :] <= np.arange(128)[:, None], 0.0, NEG).astype(np.float32)
    invf = (500000.0 ** (-np.arange(0, 16, 2, dtype=np.float32) / 16)).astype(np.float32)
    invf = np.broadcast_to(invf[None, :], (128, 8)).copy()
    pow2 = np.broadcast_to((0.5 ** np.arange(1, NIT + 1))[None, :], (128, NIT)).astype(np.float32).copy()
    return {"c_ident": ident, "c_triu": triu, "c_cbias": cbias, "c_invf": invf, "c_pow2": pow2}


def brow(v, n):
    return v.re("(o n) -> o n", o=1).bc([128, n])


class K:
    pass


def build(debug=False, stop_after=None, layers=DEPTH):
    nc = bass.Bass("TRN2", target_bir_lowering=False)
    P = Prog(nc)
    k = K()
    k.P = P
    k.debug = debug
    k.skip_abc = stop_after if stop_after in ("M1only", "M2only", "CM1", "BCM1", "AM1") else None
    import os
    k.no_router = bool(os.environ.get("NO_ROUTER"))
    k.stop_m1 = (stop_after == "M1")
    if stop_after == "M1":
        stop_after = "M"
    dk = "ExternalOutput" if debug else "Internal"
    k.x = P.dram("x", [SEQ, D], F32, kind="ExternalInput")
    k.pos = P.dram("pos", [128, NT], I32, kind="ExternalInput")
    k.w = {}
    for n, shp in WNAMES:
        k.w[n] = P.dram(n, shp, F32, kind="ExternalInput")
    k.c = {}
    for n, a in host_consts().items():
        k.c[n] = P.dram(n, list(a.shape), F32, kind="ExternalInput")
    k.out = P.dram("out", [SEQ, D], F32, kind="ExternalOutput")
    k.hres = P.dram("hres", [SEQ, D], F32, kind=dk)
    k.YA = P.dram("YA", [NT, 128, 4, 128], BF16, kind=dk)
    k.YB = P.dram("YB", [NT, 128, 4, 128], BF16, kind=dk)
    k.YC = P.dram("YC", [NT, 128, 4, 128], BF16, kind=dk)
    k.hres_b = [Buf("hres%d" % t) for t in range(NT)]
    k.HM = P.dram("HM", [NT, 128, D], BF16, kind=dk)
    k.HM_b = [Buf("HM%d" % t) for t in range(NT)]
    k.Y_b = {n: [Buf("%s%d" % (n, t)) for t in range(NT)] for n in ("YA", "YB", "YC")}

    hT_all = P.sbuf("hT", [128, 8, SEQ], BF16, glob=True)
    k.hT_b = [Buf("hT%d" % t) for t in range(NT)]
    k.hT_all = hT_all
    k.ident = P.sbuf("ident", [128, 128], F32, glob=True)
    k.identb = P.sbuf("identb", [128, 128], BF16, glob=True)
    k.triu = P.sbuf("triu", [128, 128], F32, glob=True)
    k.cbias = P.sbuf("cbias", [128, 128], F32, glob=True)
    k.ones = P.sbuf("ones", [128, 128], F32, glob=True)
    k.onesb = P.sbuf("onesb", [128, 128], BF16, glob=True)
    k.eps5 = P.sbuf("eps5", [128, 1], F32, glob=True)
    k.eps6 = P.sbuf("eps6", [128, 1], F32, glob=True)
    k.COS = P.sbuf("COS", [128, NT, 8], F32, glob=True)
    k.SIN = P.sbuf("SIN", [128, NT, 8], F32, glob=True)
    k.COMB = P.sbuf("COMB", [128, NT, 32], F32, glob=True)
    k.pow2 = P.sbuf("pow2", [128, NIT], F32, glob=True)

    phase0(k)
    if stop_after == "p0":
        return fin(k)
    for l in range(layers):
        if k.skip_abc:
            if k.skip_abc == "M1only":
                phaseM1(k, l)
            if k.skip_abc == "M2only":
                phaseM2(k, l)
            if k.skip_abc == "CM1":
                phaseC(k, l); phaseM1(k, l)
            if k.skip_abc == "BCM1":
                phaseB(k, l); phaseC(k, l); phaseM1(k, l)
            if k.skip_abc == "AM1":
                phaseA(k, l); phaseM1(k, l)
            return fin(k)
        phaseA(k, l)
        if stop_after == "A":
            return fin(k)
        phaseB(k, l)
        if stop_after == "B":
            return fin(k)
        phaseC(k, l)
        if stop_after == "C":
            return fin(k)
        phaseM(k, l)
        if stop_after == "M":
            return fin(k)
        phaseE(k, l, last=(l == layers - 1))
    return fin(k)


def fin(k):
    k.P.finish()
    return k.P.nc, k.P


def hT(k, t):
    return V(k.hT_all.ap[:, :, t * 128:(t + 1) * 128], k.hT_b[t])


def hT_span(k, dc, t0, nt):
    return V(k.hT_all.ap[:, dc, t0 * 128:(t0 + nt) * 128], tuple(k.hT_b[t0:t0 + nt]))


def hres_tile(k, t):
    return V(k.hres.ap[t * 128:(t + 1) * 128, :], k.hres_b[t])


def load_w(P, dst, src, q="pool", engs=("pool",), nst=3):
    n = src.ap.shape[1]
    import os
    if not USE_SWDGE:
        if not hasattr(P, "_stage") or P._stage_phase != P.nphase:
            P._stage = P.sbuf("wstage", [128, nst, 1024], F32, nslots=nst)
            P._stage_phase = P.nphase
            P._stage_i = 0
        nch = src.ap.shape[0] // 128
        ns = len(P._stage)
        if n < 1024 and 1024 % n == 0 and nch % (1024 // n) == 0 and len(dst.ap.shape) == 3:
            G = 1024 // n
            for c in range(0, nch, G):
                st = P._stage[P._stage_i % ns]
                ce = engs[P._stage_i % len(engs)]
                P._stage_i += 1
                P.dma("sp", st.re("p (g n) -> p g n", g=G), src[c * 128:(c + G) * 128, :].re("(g p) n -> p g n", p=128), sb=st)
                P.copy(ce, dst[:, c:c + G, :], st.re("p (g n) -> p g n", g=G))
            return
        for c0 in range(0, n, 1024):
            for c in range(nch):
                c1 = min(n, c0 + 1024)
                st = P._stage[P._stage_i % ns]
                ce = engs[P._stage_i % len(engs)]
                P._stage_i += 1
                P.dma("sp", st[:, 0:c1 - c0], src[c * 128:(c + 1) * 128, c0:c1], sb=st)
                P.copy(ce, dst[:, c, c0:c1], st[:, 0:c1 - c0])
        return
    for c0 in range(0, n, 1024):
        c1 = min(n, c0 + 1024)
        P.dma(q, dst[:, :, c0:c1], src[:, c0:c1].re("(c p) n -> p c n", p=128), sb=dst)


class LN:
    def __init__(self, k, name, Dn, eps_t, nb=2):
        P = k.P
        self.k = k
        self.Dn = Dn
        self.nch = max(1, Dn // 512)
        self.cw = min(Dn, 512)
        self.st = P.sbuf(name + "_st", [128, nb, self.nch * 6], F32, nslots=nb)
        self.mv = P.sbuf(name + "_mv", [128, nb, 2], F32, nslots=nb)
        self.lv = P.sbuf(name + "_lv", [128, nb, 1], F32, nslots=nb)
        self.rs = P.sbuf(name + "_rs", [128, nb, 1], F32, nslots=nb)
        self.eps = eps_t
        self.nb = nb
        self.i = 0

    def __call__(self, r, y, gbc=None, bbc=None, eng_aff="pool", eng_bias=None):
        self.part1(r, y)
        self.part2(y, gbc, bbc, eng_aff, eng_bias)

    def part2(self, y, gbc=None, bbc=None, eng_aff="pool", eng_bias=None):
        P = self.k.P
        if gbc is not None:
            P.tt(eng_aff, y, y, gbc, ALU.mult)
        if bbc is not None:
            P.tt(eng_bias or eng_aff, y, y, bbc, ALU.add)

    def part1(self, r, y):
        P = self.k.P
        j = self.i % self.nb
        self.i += 1
        st, mv, lv, rs = self.st[j], self.mv[j], self.lv[j], self.rs[j]
        for c in range(self.nch):
            P.generic("dve", lambda e, c=c: e.bn_stats(st.ap[:, c * 6:(c + 1) * 6], r.ap[:, c * self.cw:(c + 1) * self.cw]),
                      [r], [st])
        P.generic("dve", lambda e: e.bn_aggr(mv.ap, st.ap), [st], [mv])
        P.act(lv, mv[:, 1:2], AF.Ln, bias=self.eps, scale=1.0)
        P.act(rs, lv, AF.Exp, scale=-0.5)
        P.ts("dve", y, r, mv[:, 0:1], rs, op0=ALU.subtract, op1=ALU.mult)


def phase0(k):
    P = k.P
    P.begin("p0")
    for name, t in (("c_ident", k.ident), ("c_triu", k.triu), ("c_cbias", k.cbias), ("c_pow2", k.pow2)):
        P.dma("sp", t, k.c[name])
    P.copy("dve", k.identb, k.ident)
    P.memset("pool", k.ones, 1.0)
    P.memset("pool", k.onesb, 1.0)
    P.memset("pool", k.eps5, 1e-5)
    P.memset("pool", k.eps6, 1e-6)
    posi = P.sbuf("posi", [128, NT], I32)
    posf = P.sbuf("posf", [128, NT], F32)
    invf = P.sbuf("invf", [128, 8], F32)
    ang = P.sbuf("ang", [128, NT, 8], F32)
    a2 = P.sbuf("a2", [128, NT, 8], F32)
    kf = P.sbuf("kf", [128, NT, 8], F32)
    ki = P.sbuf("ki", [128, NT, 8], I32)
    P.dma("sp", posi, k.pos)
    P.dma("sp", invf, k.c["c_invf"])
    P.copy("dve", posf, posi)
    P.tt("dve", ang, posf.un(2).bc([128, NT, 8]), invf.un(1).bc([128, NT, 8]), ALU.mult)
    for tab, shift in ((k.SIN, 0.0), (k.COS, np.pi / 2)):
        if shift != 0.0:
            P.ts("dve", a2, ang, float(shift), None, op0=ALU.add)
            src = a2
        else:
            src = ang
        P.ts("dve", kf, src, float(1.0 / TWO_PI), None, op0=ALU.mult)
        P.copy("dve", ki, kf)
        P.copy("dve", kf, ki)
        P.stt("dve", a2, kf, float(-C1), src, ALU.mult, ALU.add)
        P.stt("dve", a2, kf, float(-C2), a2, ALU.mult, ALU.add)
        P.ts("dve", a2, a2, float(np.pi), float(-np.pi), op0=ALU.min, op1=ALU.max)
        P.act(tab, a2, AF.Sin)
    gbc = P.sbuf("gin", [128, D], F32)
    bbc = P.sbuf("bin", [128, D], F32)
    P.dma("sp", gbc, brow(k.w["ln_in_g"], D))
    P.dma("sp", bbc, brow(k.w["ln_in_b"], D))
    xt = P.sbuf("xt", [128, 2, D], F32, nslots=2)
    yt = P.sbuf("yt", [128, 2, D], F32, nslots=2)
    pT = P.psum("pT", [128, 2, 8, 128], F32, nslots=2)
    ln = LN(k, "ln0", D, k.eps5)
    P.dma("sp", xt[0], k.x[0:128, :])
    P.dma("sp", xt[1], k.x[128:256, :])
    ln.part1(xt[0], yt[0])
    for t in range(NT):
        j = t % 2
        if t + 1 < NT:
            ln.part1(xt[(t + 1) % 2], yt[(t + 1) % 2])
        if t + 2 < NT:
            P.dma("sp", xt[j], k.x[(t + 2) * 128:(t + 3) * 128, :])
        ln.part2(yt[j], gbc, bbc, "pool", "dve")
        P.dma("sp", hres_tile(k, t), yt[j], sb=yt[j])
        for c in range(8):
            P.tr(pT[j][:, c, :], yt[j][:, c * 128:(c + 1) * 128], k.ident)
        P.copy("act", hT(k, t), pT[j])
    P.end()


def phaseA(k, l):
    P = k.P
    P.begin("A%d" % l)
    w_in = k.w["w_in"][l]
    WA = P.sbuf("WA", [128, 8, 964], BF16)
    load_w(P, WA, w_in[:, 0:964], engs=("pool", "act", "dve"))
    gik = P.sbuf("gik", [128, 64], F32)
    P.dma("sp", gik, brow(k.w["idx_k_g"][l], 64))
    ident4 = P.sbuf("ident4", [128, 4, 128], BF16)
    P.copy("pool", ident4, k.identb.un(1).bc([128, 4, 128]))
    id4 = ident4.re("p a b -> p (a b)")
    kT = P.sbuf("kT", [64, SEQ], BF16)
    kiT = P.sbuf("kiT", [64, SEQ], BF16)
    kT_b = [Buf("kT%d" % t) for t in range(NT)]
    kiT_b = [Buf("kiT%d" % t) for t in range(NT)]
    V1 = P.sbuf("V1", [128, NT, 65], BF16)
    V1_b = [Buf("V1_%d" % t) for t in range(NT)]
    P.memset("pool", V1.wb(tuple(V1_b)), 1.0)
    X = P.sbuf("X", [128, 2, 964], F32, nslots=2)
    Xr = P.sbuf("Xr", [128, 2, 960], BF16, nslots=2)
    rt = P.sbuf("rt", [128, 2, 4, 15, 8], F32, nslots=2)
    wsc = P.sbuf("wsc", [128, 2, 4], F32, nslots=2)
    lns = P.sbuf("lns", [128, 2, 8], F32, nslots=2)
    j64 = P.sbuf("j64", [128, 64], F32)
    qTb = P.sbuf("qTb", [64, 3, 1024], BF16, nslots=3)
    qiTb = P.sbuf("qiTb", [64, 2, 512], BF16, nslots=2)
    S = P.sbuf("S", [128, 2, SEQ], F32, nslots=2)
    rl = P.sbuf("rl", [128, 3, 512], F32, nslots=3)
    mb = P.sbuf("mb", [128, 2, SEQ], BF16, nslots=2)
    bs = P.sbuf("bs", [128, 2, 8], F32, nslots=2)
    cn = P.sbuf("cn", [128, 2, NIT], F32, nslots=2)
    halves = P.sbuf("halves", [128, 2, NIT], F32, nslots=2)
    E = P.sbuf("E", [128, 2, 1024], BF16, nslots=2)
    rec = P.sbuf("rec", [128, 2, 8], F32, nslots=2)
    ya = P.sbuf("ya", [128, 2, 512], BF16, nslots=2)
    yaT = P.sbuf("yaT", [128, 2, 512], BF16, nslots=2)
    pAS = P.psum("pAS", [128, 2, 512], F32, nslots=2)
    pT = P.psum("pT", [128, 16, 128], BF16)
    pL = P.psum("pL", [128, 2, 512], F32, nslots=2)
    pO = P.psum("pO", [128, 2, 512], F32, nslots=2)
    cnt = [0]

    def front_units(qb):
        j = qb % 2
        nk = (qb + 1) * 128
        Xj, Xrj = X[j], Xr[j]
        X3 = Xj[:, 0:960].re("p (s d) -> p s d", d=64)
        R3 = Xrj.re("p (s d) -> p s d", d=64)
        Sj = S[j]
        units = []

        def u_proj():
            for (ps, c0, c1) in ((pAS[0], 0, 512), (pAS[1], 512, 964)):
                for dc in range(8):
                    P.mm(ps[:, 0:c1 - c0], hT(k, qb)[:, dc, :], WA[:, dc, c0:c1], start=(dc == 0), stop=(dc == 7))
                P.copy("act", Xj[:, c0:c1], ps[:, 0:c1 - c0])
        units.append(u_proj)

        def u_ln_rope():
            xs = Xj[:, 896:960]
            ls = lns[j]
            s1, s2, m2, bb, lv, rs, nm, nmr = (ls[:, i:i + 1] for i in range(8))
            P.memset("pool", ls[:, 0:2], 0.0)
            P.act(j64, xs, AF.Identity, accum=s1)
            P.act(j64, xs, AF.Square, accum=s2)
            P.act(m2, s1, AF.Square, scale=1.0 / 64)
            P.act(bb, m2, AF.Identity, bias=k.eps5, scale=-1.0)
            P.act(lv, s2, AF.Ln, bias=bb, scale=1.0 / 64)
            P.act(rs, lv, AF.Exp, scale=-0.5)
            P.act(nm, s1, AF.Identity, scale=-1.0 / 64)
            P.act(nmr, nm, AF.Identity, scale=rs)
            P.act(xs, xs, AF.Identity, bias=nmr, scale=rs)
            P.tt("pool", xs, xs, gik, ALU.mult)
            P._rec("pool", lambda e: e.tensor_copy(V1.ap[:, qb, 0:64], Xj.ap[:, 576:640]), [Xj], [V1_b[qb]])
            P.ts("pool", wsc[j], Xj[:, 960:964], 0.0625, None, op0=ALU.mult)
            cb = k.COS[:, qb, :].un(1).bc([128, 15, 8])
            sb_ = k.SIN[:, qb, :].un(1).bc([128, 15, 8])
            t1, t2, t3, t4 = (rt[j][:, i] for i in range(4))
            P.copy("pool", R3[:, :, 16:64], X3[:, :, 16:64])
            P.tt("pool", t1, X3[:, :, 0:8], cb, ALU.mult)
            P.tt("pool", t2, X3[:, :, 8:16], sb_, ALU.mult)
            P.tt("pool", t3, X3[:, :, 0:8], sb_, ALU.mult)
            P.tt("pool", t4, X3[:, :, 8:16], cb, ALU.mult)
            P.tt("pool", R3[:, :, 0:8], t1, t2, ALU.subtract)
            P.tt("pool", R3[:, :, 8:16], t3, t4, ALU.add)
        units.append(u_ln_rope)

        def u_tr():
            slots = list(range(0, 9)) + list(range(10, 15))
            for i, s in enumerate(slots):
                P.tr(pT[0:64, i, :], R3[:, s, :], k.identb)
            P.copy("act", qTb[qb % 3], pT[0:64, 0:8, :].re("p a b -> p (a b)"))
            P.copy("act", V(kT.ap[:, qb * 128:(qb + 1) * 128], kT_b[qb]), pT[0:64, 8, :])
            P.copy("act", qiTb[j], pT[0:64, 9:13, :].re("p a b -> p (a b)"))
            P.copy("act", V(kiT.ap[:, qb * 128:(qb + 1) * 128], kiT_b[qb]), pT[0:64, 13, :])
        units.append(u_tr)

        nch = (nk + 511) // 512
        for ch in range(nch):
            for h in range(4):
                def u_s(ch=ch, h=h):
                    kw = min(512, nk - ch * 512)
                    tb = tuple(kiT_b[ch * 4:ch * 4 + kw // 128])
                    sl_ = Sj[:, ch * 512:ch * 512 + kw]
                    ps = pAS[cnt[0] % 2]
                    r_ = rl[cnt[0] % 3]
                    cnt[0] += 1
                    P.mm(ps[:, 0:kw], qiTb[j][:, h * 128:(h + 1) * 128], V(kiT.ap[:, ch * 512:ch * 512 + kw], tb))
                    P.act(r_[:, 0:kw], ps[:, 0:kw], AF.Relu)
                    wb_ = wsc[j][:, h:h + 1].bc([128, kw])
                    if h == 0:
                        P.tt("pool", sl_, r_[:, 0:kw], wb_, ALU.mult)
                    else:
                        P.tt("pool", r_[:, 0:kw], r_[:, 0:kw], wb_, ALU.mult)
                        P.tt("pool", sl_, sl_, r_[:, 0:kw], ALU.add)
                    if ch == nch - 1 and h == 3:
                        P.tt("pool", Sj[:, qb * 128:nk], Sj[:, qb * 128:nk], k.cbias, ALU.add)
                units.append(u_s)
        return units

    def thr(qb):
        j = qb % 2
        nk = (qb + 1) * 128
        Sj, mk = S[j], mb[j]
        b_ = bs[j]
        lo, hi, rng, tr_, d_, th = (b_[:, i:i + 1] for i in range(6))
        if qb >= 2:
            nd = qb * 128
            P.reduce("dve", hi, Sj[:, 0:nd], ALU.max)
            P.reduce("dve", lo, Sj[:, 0:nd], ALU.min)
            P.tt("dve", rng, hi, lo, ALU.subtract)
            P.ts("dve", halves[j], k.pow2, rng, None, op0=ALU.mult)
            P.tt("dve", tr_, lo, halves[j][:, 0:1], ALU.add)
            P.memset("dve", cn[j], 0.0)
            for it in range(NIT):
                c_ = cn[j][:, it:it + 1]
                h_ = halves[j][:, it:it + 1]
                P.ts("dve", mk[:, 0:nk], Sj[:, 0:nk], tr_, 0.0, op0=ALU.is_ge, op1=ALU.add, accum=c_)
                P.ts("dve", d_, c_, 255.5, 0.5, op0=ALU.is_ge, op1=ALU.subtract)
                P.stt("dve", tr_, d_, h_, tr_, ALU.mult, ALU.add)
            P.stt("dve", th, halves[j][:, NIT - 1:NIT], -0.5, tr_, ALU.mult, ALU.add)
        else:
            P.memset("dve", th, -1.0e29)
        P.ts("dve", mk[:, 0:nk], Sj[:, 0:nk], th, -30000.0, op0=ALU.is_lt, op1=ALU.mult)

    def att_units(qb):
        j = qb % 2
        mk = mb[j]
        units = []

        def pv(kc):
            v1 = V(V1.ap[:, kc, :], V1_b[kc])
            for h in range(8):
                P.mm(pO[h // 4][:, (h % 4) * 65:(h % 4) * 65 + 65], E[kc % 2][:, h * 128:(h + 1) * 128], v1,
                     start=(kc == 0 and h % 4 == 0), stop=(kc == qb and h % 4 == 3))

        for kc in range(qb + 1):
            def u(kc=kc):
                kTv = V(kT.ap[:, kc * 128:(kc + 1) * 128], kT_b[kc])
                for hh in range(2):
                    P.mm(pL[hh], kTv, qTb[qb % 3][:, hh * 512:(hh + 1) * 512], start=True, stop=False)
                    P.mm(pL[hh], mk[:, kc * 128:(kc + 1) * 128], id4, start=False, stop=True)
                    P.act(E[kc % 2][:, hh * 512:(hh + 1) * 512], pL[hh], AF.Exp, scale=0.125)
                if kc > 0:
                    pv(kc - 1)
                if kc == qb:
                    pv(kc)
            units.append(u)
        return units

    def norm(qb):
        j = qb % 2
        for hh in range(2):
            o3 = pO[hh][:, 0:260].re("p (h d) -> p h d", d=65)
            rc = rec[j][:, hh * 4:(hh + 1) * 4]
            P.generic("dve", lambda e, rc=rc, o3=o3: e.reciprocal(rc.ap, o3.ap[:, :, 64]), [o3], [rc])
            P.tt("dve", ya[j][:, hh * 256:(hh + 1) * 256].re("p (h d) -> p h d", d=64), o3[:, :, 0:64],
                 rc.un(2).bc([128, 4, 64]), ALU.mult)
        for c in range(4):
            P.tr(pT[:, c, :], ya[j][:, c * 128:(c + 1) * 128], k.identb)
        P.copy("act", yaT[j], pT[:, 0:4, :].re("p a b -> p (a b)"))
        P.dma("sp", V(k.YA.ap[qb].rearrange("p a b -> p (a b)"), k.Y_b["YA"][qb]), yaT[j], sb=yaT[j])

    def run_merged(a, b):
        import os
        if os.environ.get("A_INTERLEAVE") == "0":
            for f in b:
                f()
            for f in a:
                f()
            return
        ia = ib = 0
        while ia < len(a) or ib < len(b):
            if ib >= len(b) or (ia < len(a) and ia * len(b) <= ib * len(a)):
                a[ia]()
                ia += 1
            else:
                b[ib]()
                ib += 1

    run_merged(front_units(0), [])
    thr(0)
    run_merged(front_units(1), [])
    for i in range(NT):
        if i + 1 < NT:
            thr(i + 1)
        run_merged(front_units(i + 2) if i + 2 < NT else [], att_units(i))
        norm(i)
    P.end()


def phaseB(k, l):
    P = k.P
    P.begin("B%d" % l)
    w_in = k.w["w_in"][l]
    WB = P.sbuf("WB", [128, 8, 1552], BF16)
    load_w(P, WB[:, :, 0:1024], w_in[:, 964:1988], engs=("pool", "act", "dve"), nst=6)
    load_w(P, WB[:, :, 1024:1536], w_in[:, 2004:2516], engs=("pool", "act", "dve"))
    load_w(P, WB[:, :, 1536:1552], w_in[:, 1988:2004], engs=("pool", "act", "dve"))
    wa2 = P.sbuf("wa2", [16, 256], BF16)
    wa2f = P.sbuf("wa2f", [16, 256], F32)
    P.dma("sp", wa2f, k.w["gla_wa2"][l])
    P.copy("pool", wa2, wa2f)
    ba = P.sbuf("ba", [1, 256], BF16)
    baf = P.sbuf("baf", [1, 256], F32)
    P.dma("sp", baf, k.w["gla_ba"][l].re("(o n) -> o n", o=1))
    P.copy("pool", ba, baf)
    gng = P.sbuf("gng", [128, 128], F32)
    P.dma("sp", gng, brow(k.w["gla_norm_g"][l], 128))
    triub = P.sbuf("triub", [128, 128], BF16)
    P.copy("dve", triub, k.triu)
    St = P.sbuf("St", [64, 4, 128], F32)
    Sb = P.sbuf("Sb", [64, 2, 4, 128], BF16, nslots=2)
    glT = P.sbuf("glT", [16, 2, 128], BF16, nslots=2)
    e1 = P.sbuf("e1", [128, 2, 256], F32, nslots=2)
    lg = P.sbuf("lg", [128, 2, 256], F32, nslots=2)
    bsb = P.sbuf("bsb", [128, 2, 256], F32, nslots=2)
    dd = P.sbuf("dd", [128, 2, 256], F32, nslots=2)
    eq = P.sbuf("eq", [128, 2, 256], F32, nslots=2)
    ek = P.sbuf("ek", [128, 2, 256], F32, nslots=2)
    ekl = P.sbuf("ekl", [128, 2, 256], F32, nslots=2)
    qt = P.sbuf("qt", [128, 2, 256], BF16, nslots=2)
    kt = P.sbuf("kt", [128, 2, 256], BF16, nslots=2)
    kh = P.sbuf("kh", [128, 2, 256], BF16, nslots=2)
    vb = P.sbuf("vb", [128, 2, 512], BF16, nslots=2)
    dec = P.sbuf("dec", [64, 2, 4], F32, nslots=2)
    qkT = P.sbuf("qkT", [64, 2, 1024], BF16, nslots=2)
    att = P.sbuf("att", [128, 2, 512], BF16, nslots=2)
    ss = P.sbuf("ss", [128, 2, 4], F32, nslots=2)
    sl = P.sbuf("sl", [128, 2, 4], F32, nslots=2)
    ri = P.sbuf("ri", [128, 2, 4], F32, nslots=2)
    sq = P.sbuf("sq", [128, 128], BF16)
    yb = P.sbuf("yb", [128, 2, 512], F32, nslots=2)
    sr = P.sbuf("sr", [128, 2, 512], F32, nslots=2)
    yb2 = P.sbuf("yb2", [128, 2, 512], BF16, nslots=2)
    ybT = P.sbuf("ybT", [128, 2, 512], BF16, nslots=2)
    pQA = P.psum("pQA", [128, 512], F32)
    pV = P.psum("pV", [128, 512], F32)
    pR = P.psum("pR", [128, 512], F32)
    pZG = P.psum("pZG", [128, 512], F32)
    pCS = P.psum("pCS", [128, 512], F32)
    pTr = P.psum("pTr", [128, 8, 128], BF16)
    pOo = P.psum("pOo", [128, 512], F32)
    pX = P.psum("pX", [128, 512], F32)

    def front_units(t):
        j = t % 2
        h_ = hT(k, t)

        def u1():
            for dc in range(8):
                P.mm(pQA, h_[:, dc, :], WB[:, dc, 0:512], start=(dc == 0), stop=(dc == 7))
            for dc in range(8):
                P.mm(pV, h_[:, dc, :], WB[:, dc, 512:1024], start=(dc == 0), stop=(dc == 7))
            for dc in range(8):
                P.mm(pR, h_[:, dc, :], WB[:, dc, 1024:1536], start=(dc == 0), stop=(dc == 7))
            for dc in range(8):
                P.mm(pZG[0:16, 256:384], WB[:, dc, 1536:1552], h_[:, dc, :], start=(dc == 0), stop=(dc == 7))
            P.copy("act", glT[j], pZG[0:16, 256:384])

        def u2():
            P.mm(pZG[:, 0:256], glT[j], wa2, start=True, stop=False)
            P.mm(pZG[:, 0:256], k.onesb[0:1, :], ba, start=False, stop=True)
            P.act(e1[j], pZG[:, 0:256], AF.Exp, scale=-1.0)
            P.act(lg[j], e1[j], AF.Ln, bias=k.ones[:, 0:1], scale=1.0)

        def u3():
            P.mm(pCS[:, 0:256], k.triu, lg[j])
            P.mm(pCS[:, 256:512], k.ones, lg[j])
            for h in range(4):
                P.mm(pZG[0:64, 384 + h:385 + h], lg[j][:, h * 64:(h + 1) * 64], k.ones[:, 0:1])
            P.act(dec[j], pZG[0:64, 384:388], AF.Exp, scale=-1.0 / 16)
            P.act(eq[j], pCS[:, 0:256], AF.Exp, scale=-1.0 / 16)
            P.act(ek[j], pCS[:, 0:256], AF.Exp, scale=1.0 / 16)
            P.copy("act", bsb[j], pCS[:, 0:256])
            P.tt("dve", dd[j], pCS[:, 256:512], bsb[j], ALU.subtract)
            P.act(ekl[j], dd[j], AF.Exp, scale=-1.0 / 16)
            P.stt("dve", qt[j], pQA[:, 0:256], 0.125, eq[j], ALU.mult, ALU.mult)
            P.tt("dve", kt[j], pQA[:, 256:512], ek[j], ALU.mult)
            P.tt("dve", kh[j], pQA[:, 256:512], ekl[j], ALU.mult)
            P.copy("act", vb[j], pV)
            P.act(sr[j], pR, AF.Silu)
        return [u1, u2, u3]

    def back_units(t):
        j = t % 2

        def v1():
            for h in range(4):
                P.tr(pTr[0:64, h, :], qt[j][:, h * 64:(h + 1) * 64], k.identb)
                P.tr(pTr[0:64, 4 + h, :], kt[j][:, h * 64:(h + 1) * 64], k.identb)
            P.copy("act", qkT[j], pTr[0:64, :, :].re("p a b -> p (a b)"))

        def v2():
            for h in range(4):
                P.mm(pX[:, h * 128:(h + 1) * 128], qkT[j][:, (4 + h) * 128:(5 + h) * 128], qkT[j][:, h * 128:(h + 1) * 128])
            P.tt("dve", att[j].re("p (h t) -> p h t", h=4), pX.re("p (h t) -> p h t", h=4),
                 triub.un(1).bc([128, 4, 128]), ALU.mult)

        def v3():
            Sprev = Sb[(t + 1) % 2]
            for h in range(4):
                P.mm(pOo[:, h * 128:(h + 1) * 128], att[j][:, h * 128:(h + 1) * 128], vb[j][:, h * 128:(h + 1) * 128],
                     start=True, stop=(t == 0))
                if t > 0:
                    P.mm(pOo[:, h * 128:(h + 1) * 128], qkT[j][:, h * 128:(h + 1) * 128], Sprev[:, h, :],
                         start=False, stop=True)
            if t < NT - 1:
                pSt = pX[0:64, :]
                for h in range(4):
                    P.mm(pSt[:, h * 128:(h + 1) * 128], kh[j][:, h * 64:(h + 1) * 64], vb[j][:, h * 128:(h + 1) * 128])
                if t == 0:
                    P.copy("dve", St, pSt.re("p (h v) -> p h v", h=4))
                else:
                    for h in range(4):
                        P.stt("dve", St[:, h, :], St[:, h, :], dec[j][:, h:h + 1], pSt[:, h * 128:(h + 1) * 128],
                              ALU.mult, ALU.add)
                P.copy("act", Sb[j], St)

        def v4():
            P.memset("pool", ss[j], 0.0)
            for h in range(4):
                P.act(sq, pOo[:, h * 128:(h + 1) * 128], AF.Square, accum=ss[j][:, h:h + 1])
            P.act(sl[j], ss[j], AF.Ln, bias=k.eps6, scale=1.0 / 128)
            P.act(ri[j], sl[j], AF.Exp, scale=-0.5)
            P.tt("dve", yb[j].re("p (h v) -> p h v", h=4), pOo.re("p (h v) -> p h v", h=4),
                 ri[j].un(2).bc([128, 4, 128]), ALU.mult)
            P.tt("pool", yb[j].re("p (h v) -> p h v", h=4), yb[j].re("p (h v) -> p h v", h=4),
                 gng.un(1).bc([128, 4, 128]), ALU.mult)
            P.tt("pool", yb2[j], yb[j], sr[j], ALU.mult)

        def v5():
            for c in range(4):
                P.tr(pTr[:, c, :], yb2[j][:, c * 128:(c + 1) * 128], k.identb)
            P.copy("act", ybT[j], pTr[:, 0:4, :].re("p a b -> p (a b)"))
            P.dma("sp", V(k.YB.ap[t].rearrange("p a b -> p (a b)"), k.Y_b["YB"][t]), ybT[j], sb=ybT[j])
        return [v1, v2, v3, v4, v5]

    def run_merged(a, b):
        ia = ib = 0
        while ia < len(a) or ib < len(b):
            if ib >= len(b) or (ia < len(a) and ia * len(b) <= ib * len(a)):
                a[ia]()
                ia += 1
            else:
                b[ib]()
                ib += 1

    run_merged(front_units(0), [])
    for t in range(NT):
        run_merged(front_units(t + 1) if t + 1 < NT else [], back_units(t))
    P.end()


def phaseC(k, l):
    P = k.P
    P.begin("C%d" % l)
    w_in = k.w["w_in"][l]
    WC = P.sbuf("WC", [128, 8, 1024], BF16)
    load_w(P, WC, w_in[:, 2516:3540], engs=("pool", "act", "dve"), nst=6)
    gg = P.sbuf("gg", [128, 512], F32)
    gb = P.sbuf("gb", [128, 512], F32)
    P.dma("sp", gg, brow(k.w["gm_ln_g"][l], 512))
    P.dma("sp", gb, brow(k.w["gm_ln_b"][l], 512))
    wsf = P.sbuf("wsf", [128, 4, 128], F32)
    P.dma("sp", wsf, k.w["gm_ws"][l].re("g t s -> t g s"))
    bsT = P.sbuf("bsT", [128, 4], F32)
    P.dma("sp", bsT, k.w["gm_bs"][l].re("g t -> t g"), allow_slow_non_contiguous=True)
    WsT = P.sbuf("WsT", [128, 4, 128], BF16)
    pW = P.psum("pW", [128, 4, 128], F32)
    for g in range(4):
        P.tr(pW[:, g, :], wsf[:, g, :], k.ident)
    P.tt("dve", WsT, pW, k.triu.un(1).bc([128, 4, 128]), ALU.mult)
    pU = P.psum("pU", [128, 2, 512], F32, nslots=2)
    pVv = P.psum("pVv", [128, 2, 512], F32, nslots=2)
    pM = P.psum("pM", [128, 512], F32)
    pTr = P.psum("pTr", [128, 4, 128], BF16)
    u = P.sbuf("u", [128, 2, 512], F32, nslots=2)
    vv = P.sbuf("vv", [128, 2, 512], F32, nslots=2)
    vn = P.sbuf("vn", [128, 2, 512], F32, nslots=2)
    vnb = P.sbuf("vnb", [128, 2, 512], BF16, nslots=2)
    mx = P.sbuf("mx", [128, 2, 512], F32, nslots=2)
    yc = P.sbuf("yc", [128, 2, 512], BF16, nslots=2)
    ycT = P.sbuf("ycT", [128, 2, 512], BF16, nslots=2)
    ln = LN(k, "lnc", 512, k.eps5)

    def s1(t):
        j = t % 2
        h_ = hT(k, t)
        for dc in range(8):
            P.mm(pU[j], h_[:, dc, :], WC[:, dc, 0:512], start=(dc == 0), stop=(dc == 7))
        for dc in range(8):
            P.mm(pVv[j], h_[:, dc, :], WC[:, dc, 512:1024], start=(dc == 0), stop=(dc == 7))
        P.act(u[j], pU[j], AF.Gelu_apprx_tanh)
        P.act(vv[j], pVv[j], AF.Gelu_apprx_tanh)
        ln(vv[j], vn[j], gg, None)
        P.tt("pool", vnb[j], vn[j], gb, ALU.add)

    def s2(t):
        j = t % 2
        for g in range(4):
            P.mm(pM[:, g * 128:(g + 1) * 128], WsT[:, g, :], vnb[j][:, g * 128:(g + 1) * 128])
        P.tt("dve", mx[j].re("p (g d) -> p g d", g=4), pM.re("p (g d) -> p g d", g=4),
             bsT.un(2).bc([128, 4, 128]), ALU.add)
        P.tt("pool", yc[j], mx[j], u[j], ALU.mult)

    def s3(t):
        j = t % 2
        for c in range(4):
            P.tr(pTr[:, c, :], yc[j][:, c * 128:(c + 1) * 128], k.identb)
        P.copy("act", ycT[j], pTr.re("p a b -> p (a b)"))
        P.dma("sp", V(k.YC.ap[t].rearrange("p a b -> p (a b)"), k.Y_b["YC"][t]), ycT[j], sb=ycT[j])

    s1(0)
    s1(1)
    s2(0)
    for t in range(NT):
        if t + 2 < NT:
            s1(t + 2)
        if t + 1 < NT:
            s2(t + 1)
        s3(t)
    P.end()


def phaseM(k, l):
    phaseM1(k, l)
    if getattr(k, "stop_m1", False):
        return
    phaseM2(k, l)


def phaseM1(k, l):
    P = k.P
    P.begin("M1_%d" % l)
    w_in = k.w["w_in"][l]
    WG = P.sbuf("WG", [128, 8, 3072], BF16)
    load_w(P, WG, w_in[:, 3540:6612], engs=("pool", "act", "dve"), nst=5)
    WBR = P.sbuf("WBR", [128, 3, 4, 1024], BF16)
    for i, n in enumerate(("w_branch_a", "w_branch_b", "w_branch_c")):
        load_w(P, WBR[:, i], k.w[n][l], engs=("pool", "act", "dve"))
    yT = {n: P.sbuf("m" + n, [128, 2, 512], BF16, nslots=2) for n in ("YA", "YB", "YC")}
    sg = P.sbuf("sg", [128, 2, 512], F32, nslots=2)
    tmp = P.sbuf("tmp", [128, 2, 512], F32, nslots=2)
    hm = P.sbuf("hm", [128, 2, D], F32, nslots=2)
    hmb = P.sbuf("hmb", [128, 2, D], BF16, nslots=2)
    hmT = P.sbuf("hmT", [128, 2, D], BF16, nslots=2)
    pG = P.psum("pG", [128, 2, 512], F32, nslots=2)
    pP = P.psum("pP", [128, 2, 512], F32, nslots=2)
    pTb = P.psum("pTb", [128, 2, 8, 128], BF16, nslots=2)
    cnt = 0

    def prefetch(t):
        for n in ("YA", "YB", "YC"):
            P.dma("sp", yT[n][t % 2], V(getattr(k, n).ap[t].rearrange("p a b -> p (a b)"), k.Y_b[n][t]))

    def finish(t):
        j = t % 2
        P.copy("pool", hmb[j], hm[j])
        for c in range(8):
            P.tr(pTb[j][:, c, :], hmb[j][:, c * 128:(c + 1) * 128], k.identb)
        P.copy("act", hmT[j], pTb[j].re("p a b -> p (a b)"))
        P.dma("sp", V(k.HM.ap[t], k.HM_b[t]), hmT[j], sb=hmT[j])

    prefetch(0)
    for t in range(NT):
        j = t % 2
        h_ = hT(k, t)
        if t + 1 < NT:
            prefetch(t + 1)
        for xi, n in enumerate(("YA", "YB", "YC")):
            for nb in range(2):
                if t > 0 and xi == 1 and nb == 0:
                    finish(t - 1)
                pg, pp = pG[cnt % 2], pP[cnt % 2]
                s_, t_ = sg[cnt % 2], tmp[cnt % 2]
                cnt += 1
                c0 = xi * 1024 + nb * 512
                for dc in range(8):
                    P.mm(pg, h_[:, dc, :], WG[:, dc, c0:c0 + 512], start=(dc == 0), stop=(dc == 7))
                for c in range(4):
                    P.mm(pp, yT[n][j][:, c * 128:(c + 1) * 128], WBR[:, xi, c, nb * 512:(nb + 1) * 512],
                         start=(c == 0), stop=(c == 3))
                P.act(s_, pg, AF.Sigmoid)
                if xi == 0:
                    P.tt("dve", hm[j][:, nb * 512:(nb + 1) * 512], pp, s_, ALU.mult)
                else:
                    P.tt("dve", t_, pp, s_, ALU.mult)
                    P.tt("pool", hm[j][:, nb * 512:(nb + 1) * 512], hm[j][:, nb * 512:(nb + 1) * 512], t_, ALU.add)
    finish(NT - 1)
    P.end()


def phaseM2(k, l):
    P = k.P
    P.begin("M2_%d" % l)
    WO = P.sbuf("WO", [128, 8, 1024], BF16)
    load_w(P, WO, k.w["w_out"][l], engs=("pool", "act", "dve"), nst=6)
    WR = P.sbuf("WR", [128, 8, 36], F32)
    for c in range(8):
        P.dma("sp", WR[:, c, :], k.w["wr_cat"][l][c * 128:(c + 1) * 128, :], sb=WR)
    rbias = P.sbuf("rbias", [128, 36], F32)
    P.dma("sp", rbias, brow(k.w["br_cat"][l], 36))
    g1 = P.sbuf("g1", [128, D], F32)
    b1 = P.sbuf("b1", [128, D], F32)
    P.dma("sp", g1, brow(k.w["ln1_g"][l], D))
    P.dma("sp", b1, brow(k.w["ln1_b"][l], D))
    hmT = P.sbuf("hmT", [128, 3, D], BF16, nslots=3)
    hres_t = P.sbuf("hres_t", [128, 3, D], F32, nslots=3)
    r = P.sbuf("r", [128, 2, D], F32, nslots=2)
    h1 = P.sbuf("h1", [128, 2, D], F32, nslots=2)
    hTf = P.sbuf("hTf", [128, 2, D], F32, nslots=2)
    rt_ = P.sbuf("rt_", [128, 2, 128], F32, nslots=2)
    ln = LN(k, "ln1", D, k.eps5)
    pX = P.psum("pX", [128, 2, 512], F32, nslots=2)
    pT = P.psum("pT", [128, 2, 8, 128], F32, nslots=2)
    pRt = P.psum("pRt", [128, 512], F32)

    def prefetch(t):
        P.dma("sp", hmT[t % 3], V(k.HM.ap[t], k.HM_b[t]))
        P.dma("sp", hres_t[t % 3], hres_tile(k, t))

    def stage1(t):
        j = t % 2
        for nb in range(2):
            for dc in range(8):
                P.mm(pX[nb], hmT[t % 3][:, dc * 128:(dc + 1) * 128], WO[:, dc, nb * 512:(nb + 1) * 512],
                     start=(dc == 0), stop=(dc == 7))
            P.stt("dve", r[j][:, nb * 512:(nb + 1) * 512], hres_t[t % 3][:, nb * 512:(nb + 1) * 512], float(DN_ALPHA),
                  pX[nb], ALU.mult, ALU.add)
        ln.part1(r[j], h1[j])

    def stage2a(t):
        j = t % 2
        ln.part2(h1[j], g1, b1, "pool", "dve")
        P.dma("sp", hres_tile(k, t), h1[j], sb=h1[j])

    def stage2(t):
        j = t % 2
        for c in range(8):
            P.tr(pT[j][:, c, :], h1[j][:, c * 128:(c + 1) * 128], k.ident)
        for hh in range(2):
            P.copy("act", hTf[j][:, hh * 512:(hh + 1) * 512], pT[j][:, hh * 4:(hh + 1) * 4, :].re("p a b -> p (a b)"))
        for hh in range(2):
            P.copy("act", hT(k, t)[:, hh * 4:(hh + 1) * 4, :], pT[j][:, hh * 4:(hh + 1) * 4, :])

    def stage3(t):
        router(k, t, hTf[t % 2], WR, rbias, pRt, rt_[t % 2])

    prefetch(0)
    prefetch(1)
    stage1(0)
    for t in range(NT):
        stage2a(t)
        if t + 1 < NT:
            if t + 2 < NT:
                prefetch(t + 2)
            stage1(t + 1)
        stage2(t)
        if t >= 1:
            stage3(t - 1)
    stage3(NT - 1)
    P.end()


def router(k, t, hTf, WR, rbias, ps, s):
    P = k.P
    for dc in range(8):
        P.mm(ps[:, 0:36], hTf[:, dc * 128:(dc + 1) * 128], WR[:, dc, :], start=(dc == 0), stop=(dc == 7))
    lg = s[:, 0:36]
    P.tt("dve", lg, ps[:, 0:36], rbias, ALU.add)
    gl, el = s[:, 0:4], s[:, 4:36]
    gmax, gsum, gp, d21, e21, den, w1, w2 = (s[:, 40 + i:41 + i] for i in range(8))
    ge, oh, pen = s[:, 48:52], s[:, 52:56], s[:, 56:60]
    top8 = s[:, 60:68]
    elm = s[:, 68:100]
    P.reduce("dve", gmax, gl, ALU.max)
    P.ts("dve", ge, gl, gmax, None, op0=ALU.subtract)
    P.memset("dve", gsum, 0.0)
    P.act(ge, ge, AF.Exp, accum=gsum)
    P.generic("dve", lambda e: e.reciprocal(gp.ap, gsum.ap), [gsum], [gp])
    P.ts("dve", pen, gl, gmax, float(NEG), op0=ALU.is_lt, op1=ALU.mult)
    P.tt("dve", elm.re("p (g e) -> p g e", g=4), el.re("p (g e) -> p g e", g=4), pen.un(2).bc([128, 4, 8]), ALU.add)
    P.generic("dve", lambda e: e.max(top8.ap, elm.ap), [elm], [top8])
    m1, m2 = top8[:, 0:1], top8[:, 1:2]
    P.tt("dve", d21, m2, m1, ALU.subtract)
    P.act(e21, d21, AF.Exp)
    P.ts("dve", den, e21, 1.0, None, op0=ALU.add)
    P.generic("dve", lambda e: e.reciprocal(den.ap, den.ap), [den], [den])
    P.tt("dve", w1, den, gp, ALU.mult)
    P.tt("dve", w2, w1, e21, ALU.mult)
    cm = k.COMB[:, t, :]
    P.ts("dve", cm, elm, m1, w1, op0=ALU.is_equal, op1=ALU.mult)
    P.ts("dve", elm, elm, m2, w2, op0=ALU.is_equal, op1=ALU.mult)
    P.tt("dve", cm, cm, elm, ALU.add)


def phaseE(k, l, last):
    P = k.P
    P.begin("E%d" % l)
    HALF = NT // 2
    g2 = P.sbuf("g2", [128, D], F32)
    b2 = P.sbuf("b2", [128, D], F32)
    P.dma("sp", g2, brow(k.w["ln2_g"][l], D))
    P.dma("sp", b2, brow(k.w["ln2_b"][l], D))
    acc = P.sbuf("acc", [128, HALF, D], F32, nslots=HALF)
    import os
    NWS = 3 if USE_SWDGE else 2
    wg = P.sbuf("wg", [128, NWS, 8, 256], BF16, nslots=NWS)
    wu = P.sbuf("wu", [128, NWS, 8, 256], BF16, nslots=NWS)
    wd = P.sbuf("wd", [128, NWS, 2, 1024], BF16, nslots=NWS)
    sgt = P.sbuf("sgt", [128, 2, 512], BF16, nslots=2)
    hid = P.sbuf("hid", [128, 4, 512], BF16, nslots=4)
    h1t = P.sbuf("h1t", [128, 3, D], F32, nslots=3)
    ln = LN(k, "ln2", D, k.eps5)
    pGt = P.psum("pGt", [128, 2, 512], F32, nslots=2)
    pUp = P.psum("pUp", [128, 2, 512], F32, nslots=2)
    pOd = P.psum("pOd", [128, 4, 512], F32, nslots=4)
    pT = None
    w_gate, w_up, w_down = k.w["w_gate"][l], k.w["w_up"][l], k.w["w_down"][l]
    cg = [0]
    co = [0]

    def gate_up(s, tg):
        hids = []
        for fb in range(2):
            pg, pu = pGt[cg[0] % 2], pUp[cg[0] % 2]
            s_ = sgt[cg[0] % 2]
            hd = hid[cg[0] % 4]
            cg[0] += 1
            for dc in range(8):
                P.mm(pg, wg[s][:, dc, fb * 128:(fb + 1) * 128], hT_span(k, dc, tg, 4), start=(dc == 0), stop=(dc == 7))
            for dc in range(8):
                P.mm(pu, wu[s][:, dc, fb * 128:(fb + 1) * 128], hT_span(k, dc, tg, 4), start=(dc == 0), stop=(dc == 7))
            P.act(s_, pg, AF.Silu)
            P.tt("dve", hd, pu, s_, ALU.mult)
            hids.append(hd)
        return hids

    def down(s, e, tg, t0, hids):
        for ti in range(4):
            t = tg + ti
            pos = []
            for nb in range(2):
                pos.append(pOd[co[0] % 4])
                co[0] += 1
            if ti == 0:
                order = [(0, 0), (1, 0), (0, 1), (1, 1)]
            else:
                order = [(0, 0), (0, 1), (1, 0), (1, 1)]
            for nb, fb in order:
                P.mm(pos[nb], hids[fb][:, ti * 128:(ti + 1) * 128], wd[s][:, fb, nb * 512:(nb + 1) * 512],
                     start=(fb == 0), stop=(fb == 1))
            for nb in range(2):
                a_ = acc[t - t0][:, nb * 512:(nb + 1) * 512]
                cw = k.COMB[:, t, e:e + 1]
                if e == 0:
                    P.ts("dve", a_, pos[nb], cw, None, op0=ALU.mult)
                else:
                    P.stt("dve", a_, pos[nb], cw, a_, ALU.mult, ALU.add)

    for half in range(2):
        t0 = half * HALF
        for e in range(32):
            s = (half * 32 + e) % NWS
            load_w(P, wg[s], w_gate[e], engs=("pool", "act"))
            load_w(P, wu[s], w_up[e], engs=("pool", "act"))
            load_w(P, wd[s], w_down[e], engs=("pool", "act"))
            for grp in range(HALF // 4):
                tg = t0 + grp * 4
                hids = gate_up(s, tg)
                down(s, e, tg, t0, hids)
        def tail1(ti):
            t = t0 + ti
            a_ = acc[ti]
            P.stt("dve", a_, h1t[ti % 3], float(DN_ALPHA), a_, ALU.mult, ALU.add)
            ln.part1(a_, a_)

        def tail1b(ti):
            t = t0 + ti
            a_ = acc[ti]
            ln.part2(a_, g2, b2, "pool", "dve")
            if last:
                P.dma("sp", V(k.out.ap[t * 128:(t + 1) * 128, :], Buf("o%d" % t)), a_, sb=a_)
            else:
                P.dma("sp", hres_tile(k, t), a_, sb=a_)

        def tail2(ti):
            t = t0 + ti
            a_ = acc[ti]
            pt = (pOd[0], pOd[1]) if ti % 2 == 0 else (pOd[2], pOd[3])
            for c in range(8):
                P.tr(pt[c // 4][:, (c % 4) * 128:(c % 4 + 1) * 128], a_[:, c * 128:(c + 1) * 128], k.ident)
            for hh in range(2):
                P.copy("act", hT(k, t)[:, hh * 4:(hh + 1) * 4, :], pt[hh].re("p (a b) -> p a b", a=4))

        P.dma("sp", h1t[0], hres_tile(k, t0))
        P.dma("sp", h1t[1], hres_tile(k, t0 + 1))
        tail1(0)
        for ti in range(HALF):
            if ti + 2 < HALF:
                P.dma("sp", h1t[(ti + 2) % 3], hres_tile(k, t0 + ti + 2))
            if ti + 1 < HALF:
                tail1(ti + 1)
            tail1b(ti)
            if not last:
                tail2(ti)
    P.end()


_CACHE = {}


def make_in_maps(inputs, n_cores=8):
    consts = host_consts()
    inputs = dict(inputs)
    inputs["wr_cat"] = np.concatenate([np.asarray(inputs["w_rg"]), np.asarray(inputs["w_re"])], axis=-1)
    inputs["br_cat"] = np.concatenate([np.asarray(inputs["b_rg"]), np.asarray(inputs["b_re"])], axis=-1)
    shared = {n: np.ascontiguousarray(np.asarray(inputs[n], dtype=np.float32)) for n, _ in WNAMES}
    shared.update(consts)
    maps = []
    x = np.asarray(inputs["x"], dtype=np.float32)
    pos = np.asarray(inputs["positions"]).astype(np.int32)
    for b in range(n_cores):
        m = dict(shared)
        m["x"] = np.ascontiguousarray(x[b])
        m["pos"] = np.ascontiguousarray(pos[b].reshape(NT, 128).T)
        maps.append(m)
    return maps


def kernel(**inputs):
    if "nc" not in _CACHE:
        _CACHE["nc"] = build()[0]
    nc = _CACHE["nc"]
    maps = make_in_maps(inputs, 8)
    res = run_bass_kernel_spmd(nc, maps, core_ids=list(range(8)))
    return np.stack([np.asarray(r["out"], dtype=np.float32) for r in res.results], axis=0)
```
